# Optimizing a Trainium2 kernel written in Bass

```python
import math
import jax, jax.numpy as jnp
from jax import lax
import numpy as np

D_MODEL = 2048
BATCH = 1
SEQ = 16384
DEPTH = 4

GRID_W = 64
CTX_LEN = 256
HEAD_DIM = 128
N_GROUPS = 4
GROUP_W = D_MODEL // N_GROUPS
N_GROUP_HEADS = GROUP_W // HEAD_DIM
N_KV_HEADS = 2
N_REP = N_GROUP_HEADS // N_KV_HEADS
KV_W = N_KV_HEADS * HEAD_DIM
ATT_W = GROUP_W + 2 * KV_W
CONV_OFF = 0
SGU_OFF = CONV_OFF + 2 * GROUP_W
SWA_OFF = SGU_OFF + 2 * GROUP_W
GLB_OFF = SWA_OFF + ATT_W
IN_W = GLB_OFF + ATT_W
CONV_W = 31
CHUNK = 128
WINDOW = 128
BLOCK_Q = 128
ROPE_THETA = 10000.0
D_FF = 5632
N_EXPERTS = 8
TOP_K = 2
D_FF_EXPERT = 5632
MOE_BLOCK = 512
EPS = 1e-6
NEG = -1e30
SCALE = HEAD_DIM ** -0.5

kernel_name = 'hybrid_parallel_group_flow_backbone'


def rms_norm(x, g):
    xf = x.astype(jnp.float32)
    y = xf * lax.rsqrt(jnp.mean(xf * xf, axis=-1, keepdims=True) + EPS)
    return (y * g.astype(jnp.float32)).astype(x.dtype)


def layer_norm(x, g, b):
    xf = x.astype(jnp.float32)
    xc = xf - jnp.mean(xf, axis=-1, keepdims=True)
    y = xc * lax.rsqrt(jnp.mean(xc * xc, axis=-1, keepdims=True) + EPS)
    return (y * g.astype(jnp.float32) + b.astype(jnp.float32)).astype(x.dtype)


def modulate(h, shift, scale):
    return h * (1 + scale) + shift


def group_rms_norm(y, g):
    shp = y.shape
    yg = y.reshape(shp[:-1] + (N_GROUPS, GROUP_W))
    return rms_norm(yg, g.reshape(N_GROUPS, GROUP_W)).reshape(shp)


def rope_tables(n_rows):
    row = jnp.repeat(jnp.arange(n_rows, dtype=jnp.float32), GRID_W)
    col = jnp.tile(jnp.arange(GRID_W, dtype=jnp.float32), n_rows)
    axis_dim = HEAD_DIM // 2
    inv_freq = ROPE_THETA ** (-jnp.arange(0, axis_dim, 2, dtype=jnp.float32) / axis_dim)
    ang_r = row[:, None] * inv_freq[None, :]
    ang_c = col[:, None] * inv_freq[None, :]
    return (jnp.cos(ang_r), jnp.sin(ang_r), jnp.cos(ang_c), jnp.sin(ang_c))


def rope_1d(x, cos, sin):
    m = cos.shape[-1]
    x1, x2 = x[..., :m], x[..., m:]
    cc, ss = cos[:, None, :], sin[:, None, :]
    return jnp.concatenate([x1 * cc - x2 * ss, x2 * cc + x1 * ss], axis=-1)


def rope_2d(x, tabs):
    cr, sr, cc, sc = tabs
    half = HEAD_DIM // 2
    y = jnp.concatenate([rope_1d(x[..., :half], cr, sr), rope_1d(x[..., half:], cc, sc)], axis=-1)
    return y.astype(x.dtype)


def q_heads(p_q, q_g):
    B, T, _ = p_q.shape
    return rms_norm(p_q.reshape(B, T, N_GROUP_HEADS, HEAD_DIM), q_g)


def kv_heads(p_kv, k_g):
    B, T, _ = p_kv.shape
    k = rms_norm(p_kv[..., :KV_W].reshape(B, T, N_KV_HEADS, HEAD_DIM), k_g)
    v = p_kv[..., KV_W:].reshape(B, T, N_KV_HEADS, HEAD_DIM)
    return k, v


def conformer_conv(p, conv_w, conv_b, ln_g, ln_b, pw, pw_b):
    u = p[..., :GROUP_W] * jax.nn.sigmoid(p[..., GROUP_W:])
    u = lax.conv_general_dilated(u, conv_w[:, None, :], window_strides=(1,),
                                 padding=[(CONV_W // 2, CONV_W // 2)],
                                 dimension_numbers=('NWC', 'WIO', 'NWC'),
                                 feature_group_count=GROUP_W) + conv_b
    u = jax.nn.silu(layer_norm(u, ln_g, ln_b))
    return u @ pw + pw_b


def chunk_sgu(p, ln_g, ln_b, w_s, b_s):
    z = jax.nn.gelu(p)
    u, v = z[..., :GROUP_W], layer_norm(z[..., GROUP_W:], ln_g, ln_b)
    B, T, _ = v.shape
    vc = v.reshape(B, T // CHUNK, CHUNK, N_GROUP_HEADS, HEAD_DIM)
    s = jnp.einsum('gpq,bnqgc->bnpgc', w_s, vc) + b_s.T[None, None, :, :, None]
    return u * s.reshape(B, T, GROUP_W)


def ctx_attn(q, k, v, sink):
    B, T = q.shape[:2]
    qg = q.reshape(B, T, N_KV_HEADS, N_REP, HEAD_DIM)
    s = jnp.einsum('bqkgd,bskd->bkgqs', qg, k).astype(jnp.float32) * SCALE
    if sink is not None:
        sb = jnp.broadcast_to(sink.reshape(N_KV_HEADS, N_REP)[None, :, :, None, None].astype(jnp.float32),
                              s.shape[:-1] + (1,))
        s = jnp.concatenate([sb, s], axis=-1)
    pr = jax.nn.softmax(s, axis=-1)[..., -k.shape[1]:].astype(v.dtype)
    o = jnp.einsum('bkgqs,bskd->bqkgd', pr, v)
    return o.reshape(B, T, GROUP_W)


def window_attn(q, k, v, k_ctx, v_ctx, sink):
    B, L = q.shape[:2]
    nb = L // BLOCK_Q
    qb = q.reshape(B, nb, BLOCK_Q, N_KV_HEADS, N_REP, HEAD_DIM)
    pad = ((0, 0), (BLOCK_Q, BLOCK_Q), (0, 0), (0, 0))
    kp = jnp.pad(k, pad).reshape(B, nb + 2, BLOCK_Q, N_KV_HEADS, HEAD_DIM)
    vp = jnp.pad(v, pad).reshape(B, nb + 2, BLOCK_Q, N_KV_HEADS, HEAD_DIM)
    kw = jnp.concatenate([kp[:, :-2], kp[:, 1:-1], kp[:, 2:]], axis=2)
    vw = jnp.concatenate([vp[:, :-2], vp[:, 1:-1], vp[:, 2:]], axis=2)
    s_win = jnp.einsum('bnqkgd,bnskd->bnkgqs', qb, kw).astype(jnp.float32) * SCALE
    qpos = jnp.arange(nb)[:, None] * BLOCK_Q + jnp.arange(BLOCK_Q)[None, :]
    kpos = (jnp.arange(nb)[:, None] - 1) * BLOCK_Q + jnp.arange(3 * BLOCK_Q)[None, :]
    ok = ((jnp.abs(qpos[:, :, None] - kpos[:, None, :]) <= WINDOW)
          & (kpos[:, None, :] >= 0) & (kpos[:, None, :] < L))
    s_win = jnp.where(ok[None, :, None, None], s_win, NEG)
    s_ctx = jnp.einsum('bnqkgd,bckd->bnkgqc', qb, k_ctx).astype(jnp.float32) * SCALE
    sb = jnp.broadcast_to(sink.reshape(N_KV_HEADS, N_REP)[None, None, :, :, None, None].astype(jnp.float32),
                          s_ctx.shape[:-1] + (1,))
    pr = jax.nn.softmax(jnp.concatenate([sb, s_ctx, s_win], axis=-1), axis=-1)
    n_c = k_ctx.shape[1]
    p_ctx = pr[..., 1:1 + n_c].astype(v.dtype)
    p_win = pr[..., 1 + n_c:].astype(v.dtype)
    o = (jnp.einsum('bnkgqc,bckd->bnqkgd', p_ctx, v_ctx)
         + jnp.einsum('bnkgqs,bnskd->bnqkgd', p_win, vw))
    return o.reshape(B, L, GROUP_W)


def global_attn(q, k, v, k_ctx, v_ctx):
    B, L = q.shape[:2]
    nb = L // BLOCK_Q
    k_all = jnp.concatenate([k_ctx, k], axis=1)
    v_all = jnp.concatenate([v_ctx, v], axis=1)
    qb = q.reshape(B, nb, BLOCK_Q, N_KV_HEADS, N_REP, HEAD_DIM).transpose(1, 0, 2, 3, 4, 5)

    def one_block(q_blk):
        s = jnp.einsum('bqkgd,bskd->bkgqs', q_blk, k_all).astype(jnp.float32) * SCALE
        pr = jax.nn.softmax(s, axis=-1).astype(v_all.dtype)
        return jnp.einsum('bkgqs,bskd->bqkgd', pr, v_all)

    o = lax.map(one_block, qb)
    return o.transpose(1, 0, 2, 3, 4, 5).reshape(B, L, GROUP_W)


def swiglu(h, w1, w3, w2):
    return (jax.nn.silu(h @ w1) * (h @ w3)) @ w2


def moe_swiglu(h, w_r, b_r, w1, w3, w2):
    n_tok, d = h.shape
    logits = (h @ w_r + b_r).astype(jnp.float32)
    top_v, top_e = lax.top_k(logits, TOP_K)
    gates = jax.nn.softmax(top_v, axis=-1).astype(h.dtype)
    n_slot = n_tok * TOP_K
    slot_e = top_e.reshape(n_slot)
    slot_tok = jnp.arange(n_slot, dtype=jnp.int32) // TOP_K
    slot_g = gates.reshape(n_slot)
    order = jnp.argsort(slot_e)
    sorted_e = slot_e[order]
    counts = jnp.bincount(slot_e, length=N_EXPERTS).astype(jnp.int32)
    padded = (counts + MOE_BLOCK - 1) // MOE_BLOCK * MOE_BLOCK
    pad_end = jnp.cumsum(padded)
    pad_start = pad_end - padded
    start = jnp.cumsum(counts) - counts
    dest = pad_start[sorted_e] + jnp.arange(n_slot, dtype=jnp.int32) - start[sorted_e]
    n_rows = -(-(n_slot + N_EXPERTS * (MOE_BLOCK - 1)) // MOE_BLOCK) * MOE_BLOCK
    n_blocks = n_rows // MOE_BLOCK
    row_tok = jnp.full((n_rows,), n_tok, jnp.int32).at[dest].set(slot_tok[order])
    row_g = jnp.zeros((n_rows,), h.dtype).at[dest].set(slot_g[order])
    blk_e = jnp.minimum(jnp.searchsorted(pad_end, jnp.arange(n_blocks, dtype=jnp.int32) * MOE_BLOCK,
                                         side='right'), N_EXPERTS - 1)
    h_pad = jnp.concatenate([h, jnp.zeros((1, d), h.dtype)], axis=0)
    xb = h_pad[row_tok].reshape(n_blocks, MOE_BLOCK, d)

    def expert_block(args):
        x_blk, e = args
        return (jax.nn.silu(x_blk @ w1[e]) * (x_blk @ w3[e])) @ w2[e]

    yb = lax.map(expert_block, (xb, blk_e))
    y = yb.reshape(n_rows, d) * row_g[:, None]
    return jnp.zeros((n_tok + 1, d), h.dtype).at[row_tok].add(y)[:n_tok]


def setup_inputs(seed: int = 0) -> dict:
    key = jax.random.key(seed)
    ks = iter(jax.random.split(key, 48))
    f32 = jnp.float32

    def nrm(shape, scale):
        return jax.random.normal(next(ks), shape, f32) * scale

    D, G, H = D_MODEL, GROUP_W, N_GROUP_HEADS
    n_dense = (DEPTH + 1) // 2
    n_moe = DEPTH // 2
    return {
        'x': nrm((BATCH, SEQ, D), 1.0),
        'c': nrm((BATCH, D), 1.0),
        'ctx': nrm((BATCH, CTX_LEN, D), 1.0),
        'c_ctx': nrm((D,), 1.0),
        'w_ada': nrm((DEPTH, D, 6 * D), 0.5 * D ** -0.5),
        'b_ada': nrm((DEPTH, 6 * D), 0.01),
        'g_norm1': 1.0 + nrm((DEPTH, D), 0.02),
        'w_in': nrm((DEPTH, D, IN_W), D ** -0.5),
        'conv_w': nrm((DEPTH, CONV_W, G), CONV_W ** -0.5),
        'conv_b': nrm((DEPTH, G), 0.01),
        'conv_ln_g': 1.0 + nrm((DEPTH, G), 0.02),
        'conv_ln_b': nrm((DEPTH, G), 0.01),
        'conv_pw': nrm((DEPTH, G, G), G ** -0.5),
        'conv_pw_b': nrm((DEPTH, G), 0.01),
        'sgu_ln_g': 1.0 + nrm((DEPTH, G), 0.02),
        'sgu_ln_b': nrm((DEPTH, G), 0.01),
        'sgu_w': nrm((DEPTH, H, CHUNK, CHUNK), CHUNK ** -0.5),
        'sgu_b': 1.0 + nrm((DEPTH, H, CHUNK), 0.01),
        'swa_q_g': 1.0 + nrm((DEPTH, HEAD_DIM), 0.02),
        'swa_k_g': 1.0 + nrm((DEPTH, HEAD_DIM), 0.02),
        'swa_sink': nrm((DEPTH, H), 0.5),
        'glb_q_g': 1.0 + nrm((DEPTH, HEAD_DIM), 0.02),
        'glb_k_g': 1.0 + nrm((DEPTH, HEAD_DIM), 0.02),
        'g_branch': 1.0 + nrm((DEPTH, D), 0.02),
        'w_out': nrm((DEPTH, D, D), D ** -0.5),
        'g_norm2': 1.0 + nrm((DEPTH, D), 0.02),
        'ffn_w1': nrm((n_dense, D, D_FF), D ** -0.5),
        'ffn_w3': nrm((n_dense, D, D_FF), D ** -0.5),
        'ffn_w2': nrm((n_dense, D_FF, D), D_FF ** -0.5),
        'router_w': nrm((n_moe, D, N_EXPERTS), D ** -0.5),
        'router_b': nrm((n_moe, N_EXPERTS), 0.01),
        'exp_w1': nrm((n_moe, N_EXPERTS, D, D_FF_EXPERT), D ** -0.5),
        'exp_w3': nrm((n_moe, N_EXPERTS, D, D_FF_EXPERT), D ** -0.5),
        'exp_w2': nrm((n_moe, N_EXPERTS, D_FF_EXPERT, D), D_FF_EXPERT ** -0.5),
    }


def reference(x, c, ctx, c_ctx, w_ada, b_ada, g_norm1, w_in, conv_w, conv_b, conv_ln_g, conv_ln_b,
              conv_pw, conv_pw_b, sgu_ln_g, sgu_ln_b, sgu_w, sgu_b, swa_q_g, swa_k_g, swa_sink,
              glb_q_g, glb_k_g, g_branch, w_out, g_norm2, ffn_w1, ffn_w3, ffn_w2,
              router_w, router_b, exp_w1, exp_w3, exp_w2):
    B, L, D = x.shape
    n_c = ctx.shape[1]
    n_rows = L // GRID_W
    tabs = rope_tables(n_rows)
    x_lat, x_ctx = x, ctx
    for l in range(DEPTH):
        last = l == DEPTH - 1
        m_lat = jax.nn.silu(c) @ w_ada[l] + b_ada[l]
        m_ctx = jax.nn.silu(c_ctx) @ w_ada[l] + b_ada[l]
        sh1, sc1, gt1, sh2, sc2, gt2 = jnp.split(m_lat[:, None, :], 6, axis=-1)
        csh1, csc1, cgt1, csh2, csc2, cgt2 = jnp.split(m_ctx, 6)
        w = w_in[l]

        h_lat = modulate(rms_norm(x_lat, g_norm1[l]), sh1, sc1)
        h_ctx = modulate(rms_norm(x_ctx, g_norm1[l]), csh1, csc1)
        p_lat = h_lat @ w
        if last:
            w_kv = jnp.concatenate([w[:, SWA_OFF + GROUP_W:SWA_OFF + ATT_W],
                                    w[:, GLB_OFF + GROUP_W:GLB_OFF + ATT_W]], axis=1)
            p_kv = h_ctx @ w_kv
            swa_kv_c, glb_kv_c = p_kv[..., :2 * KV_W], p_kv[..., 2 * KV_W:]
        else:
            p_ctx = h_ctx @ w
            swa_kv_c = p_ctx[..., SWA_OFF + GROUP_W:SWA_OFF + ATT_W]
            glb_kv_c = p_ctx[..., GLB_OFF + GROUP_W:GLB_OFF + ATT_W]
        k_sc, v_sc = kv_heads(swa_kv_c, swa_k_g[l])
        k_gc, v_gc = kv_heads(glb_kv_c, glb_k_g[l])

        conv_args = (conv_w[l], conv_b[l], conv_ln_g[l], conv_ln_b[l], conv_pw[l], conv_pw_b[l])
        sgu_args = (sgu_ln_g[l], sgu_ln_b[l], sgu_w[l], sgu_b[l])

        y_conv = conformer_conv(p_lat[..., CONV_OFF:SGU_OFF], *conv_args)
        y_sgu = chunk_sgu(p_lat[..., SGU_OFF:SWA_OFF], *sgu_args)
        p_swa = p_lat[..., SWA_OFF:GLB_OFF]
        q_s = rope_2d(q_heads(p_swa[..., :GROUP_W], swa_q_g[l]), tabs)
        k_s, v_s = kv_heads(p_swa[..., GROUP_W:], swa_k_g[l])
        y_swa = window_attn(q_s, rope_2d(k_s, tabs), v_s, k_sc, v_sc, swa_sink[l])
        p_glb = p_lat[..., GLB_OFF:IN_W]
        q_g = rope_2d(q_heads(p_glb[..., :GROUP_W], glb_q_g[l]), tabs)
        k_g, v_g = kv_heads(p_glb[..., GROUP_W:], glb_k_g[l])
        y_glb = global_attn(q_g, rope_2d(k_g, tabs), v_g, k_gc, v_gc)
        y = group_rms_norm(jnp.concatenate([y_conv, y_sgu, y_swa, y_glb], axis=-1), g_branch[l])
        x_lat = x_lat + gt1 * (y @ w_out[l])

        if not last:
            yc_conv = conformer_conv(p_ctx[..., CONV_OFF:SGU_OFF], *conv_args)
            yc_sgu = chunk_sgu(p_ctx[..., SGU_OFF:SWA_OFF], *sgu_args)
            yc_swa = ctx_attn(q_heads(p_ctx[..., SWA_OFF:SWA_OFF + GROUP_W], swa_q_g[l]),
                              k_sc, v_sc, swa_sink[l])
            yc_glb = ctx_attn(q_heads(p_ctx[..., GLB_OFF:GLB_OFF + GROUP_W], glb_q_g[l]),
                              k_gc, v_gc, None)
            yc = group_rms_norm(jnp.concatenate([yc_conv, yc_sgu, yc_swa, yc_glb], axis=-1), g_branch[l])
            x_ctx = x_ctx + cgt1 * (yc @ w_out[l])

        h2 = modulate(rms_norm(x_lat, g_norm2[l]), sh2, sc2)
        if not last:
            h2c = modulate(rms_norm(x_ctx, g_norm2[l]), csh2, csc2)
            h2 = jnp.concatenate([h2c, h2], axis=1)
        flat = h2.reshape(-1, D)
        if l % 2 == 0:
            f = swiglu(flat, ffn_w1[l // 2], ffn_w3[l // 2], ffn_w2[l // 2])
        else:
            f = moe_swiglu(flat, router_w[l // 2], router_b[l // 2],
                           exp_w1[l // 2], exp_w3[l // 2], exp_w2[l // 2])
        f = f.reshape(B, -1, D)
        if last:
            x_lat = x_lat + gt2 * f
        else:
            x_ctx = x_ctx + cgt2 * f[:, :n_c]
            x_lat = x_lat + gt2 * f[:, n_c:]
    return x_lat
```

```python
import contextlib
import math
import numpy as np
import ml_dtypes
import concourse.bass as bass
import concourse.mybir as mybir
from concourse.bass_utils import run_bass_kernel_spmd

F32 = mybir.dt.float32
BF16 = mybir.dt.bfloat16
AF = mybir.ActivationFunctionType
ALU = mybir.AluOpType
AX = mybir.AxisListType
NPBF = ml_dtypes.bfloat16

NCORES = 8
D = 2048
KC = D // 128
SEQ = 16384
TPC = SEQ // NCORES
NCTX = 256
DEPTH = 4
GW = 512
IN_W = 4096
DFF = 5632
FC = DFF // 128
NEXP = 8
EPS = 1e-6
CONV_W = 31
HALO = 15
SCALE = 128 ** -0.5


class Res:
    __slots__ = ("last_w", "readers")

    def __init__(self):
        self.last_w = None
        self.readers = []


class Buf(Res):
    def __init__(self, t, name):
        super().__init__()
        self.t = t
        self.name = name
        self.sub = {}
        self.sem = None
        self.dma_total = 0
        self.psum = False

    def r(self, key):
        if self.psum:
            return self
        s = self.sub.get(key)
        if s is None:
            s = self.sub[key] = Res()
        return s

    def __getitem__(self, key):
        return self.t[key]


class Op:
    __slots__ = ("eng", "fn", "deps", "signal", "val", "is_dma", "dsem", "dval")

    def __init__(self, eng, fn):
        self.eng = eng
        self.fn = fn
        self.deps = []
        self.signal = False
        self.val = 0
        self.is_dma = False
        self.dsem = None
        self.dval = 0


ENGS = ("pe", "act", "dve", "pool", "sp")


class KB:
    def __init__(self, nc):
        self.nc = nc
        self.es = contextlib.ExitStack()
        self.ops = {e: [] for e in ENGS}
        self.sems = {}
        self.dma_bufs = []
        self.nbuf = 0
        self.pending = {}
        self.sem_pool = []
        self.stack = self.es
        self.stage_bufs = None

    def sem(self, name):
        return self.es.enter_context(self.nc.semaphore(name))

    def sb(self, name, shape, dtype=F32):
        self.nbuf += 1
        b = Buf(self.stack.enter_context(self.nc.sbuf_tensor(f"{name}_{self.nbuf}", list(shape), dtype)), name)
        if self.stage_bufs is not None:
            self.stage_bufs.append(b)
        return b

    def ps(self, name, shape, dtype=F32):
        self.nbuf += 1
        shape = list(shape)
        per = 1
        for d in shape[2:]:
            per *= d
        isz = 4 if dtype == F32 else 2
        shape[1] = 2048 // (isz * per)
        b = Buf(self.stack.enter_context(self.nc.psum_tensor(f"{name}_{self.nbuf}", shape, dtype)), name)
        b.psum = True
        return b

    def barrier(self):
        deps = []
        for e in ENGS:
            for o in reversed(self.ops[e]):
                if not o.is_dma:
                    deps.append(o)
                    break
        for b in self.dma_bufs:
            p = Op(None, None)
            p.is_dma = True
            p.dsem = b
            p.dval = b.dma_total
            deps.append(p)
        for e in ENGS:
            self.pending[e] = list(deps)

    @contextlib.contextmanager
    def stage(self):
        prev_stack, prev_bufs = self.stack, self.stage_bufs
        st = contextlib.ExitStack()
        self.stack, self.stage_bufs = st, []
        try:
            yield
        finally:
            self.barrier()
            for b in self.stage_bufs:
                if b.sem is not None:
                    self.sem_pool.append((b.sem, b.dma_total))
            self.stack, self.stage_bufs = prev_stack, prev_bufs
            st.close()

    def dram(self, name, shape, dtype=F32, kind="Internal"):
        return Buf(self.nc.dram_tensor(name, list(shape), dtype, kind=kind), name)

    def _deps(self, op, reads, writes, acc=False):
        deps = op.deps
        pr = [r for r in reads if getattr(r, "psum", False)]
        if pr:
            reads = [r for r in reads if not getattr(r, "psum", False)]
            writes = list(writes) + [r for r in pr if r not in writes]
        for r in reads:
            if r.last_w is not None:
                deps.append(r.last_w)
        for w in writes:
            lw = w.last_w
            if lw is not None and not (acc and lw.eng == op.eng and not lw.is_dma):
                deps.append(lw)
            for rd in w.readers:
                if rd.is_dma or rd.eng != op.eng:
                    deps.append(rd)
        for r in reads:
            r.readers.append(op)
        for w in writes:
            w.last_w = op
            w.readers = []

    def op(self, eng, fn, reads=(), writes=(), acc=False):
        o = Op(eng, fn)
        o.deps.extend(self.pending.pop(eng, ()))
        self._deps(o, reads, writes, acc)
        self.ops[eng].append(o)
        return o

    def dma(self, eng, out_ap, in_ap, semb, reads=(), writes=(), **kw):
        o = Op(eng, lambda e: e.dma_start(out=out_ap, in_=in_ap, **kw))
        o.is_dma = True
        if semb.sem is None:
            if self.sem_pool:
                semb.sem, semb.dma_total = self.sem_pool.pop()
            else:
                semb.sem = self.sem(f"d_{semb.name}_{len(self.dma_bufs)}")
            self.dma_bufs.append(semb)
        semb.dma_total += 16
        o.dsem = semb
        o.dval = semb.dma_total
        o.deps.extend(self.pending.pop(eng, ()))
        self._deps(o, reads, writes)
        self.ops[eng].append(o)
        return o

    def emit(self):
        nc = self.nc
        esem = {e: self.sem(f"e_{e}") for e in ENGS}
        for e in ENGS:
            for o in self.ops[e]:
                for d in o.deps:
                    if not d.is_dma:
                        d.signal = True
        for e in ENGS:
            c = 0
            for o in self.ops[e]:
                if o.signal and not o.is_dma:
                    c += 1
                    o.val = c
        ops = self.ops
        dma_bufs = self.dma_bufs

        def run(e, eng):
            known = {}
            for o in ops[e]:
                need = {}
                for d in o.deps:
                    if d.is_dma:
                        s, v = d.dsem.sem, d.dval
                    else:
                        s, v = esem[d.eng], d.val
                    k = id(s)
                    if k not in need or need[k][1] < v:
                        need[k] = (s, v)
                for k, (s, v) in need.items():
                    if known.get(k, 0) < v:
                        eng.wait_ge(s, v)
                        known[k] = v
                ins = o.fn(eng)
                if o.is_dma:
                    ins.then_inc(o.dsem.sem, 16)
                elif o.signal:
                    ins.then_inc(esem[e], 1)
            if e == "sp":
                for b in dma_bufs:
                    if known.get(id(b.sem), 0) < b.dma_total:
                        eng.wait_ge(b.sem, b.dma_total)

        with nc.Block() as block:
            @block.tensor
            def _(eng):
                run("pe", eng)

            @block.scalar
            def _(eng):
                run("act", eng)

            @block.vector
            def _(eng):
                run("dve", eng)

            @block.gpsimd
            def _(eng):
                run("pool", eng)

            @block.sync
            def _(eng):
                run("sp", eng)
        self.es.close()


def new_nc():
    return bass.Bass("TRN2", target_bir_lowering=False)


def bc_ap(dram_ap_1d, n, parts=128):
    return dram_ap_1d.partition_broadcast(parts)


ADA_COLS = 6 * D // NCORES


def build_ada():
    nc = new_nc()
    k = KB(nc)
    cc = k.dram("cc", [128, KC * 2], F32, kind="ExternalInput")
    w = k.dram("w", [DEPTH * D, ADA_COLS], F32, kind="ExternalInput")
    b = k.dram("b", [DEPTH, ADA_COLS], F32, kind="ExternalInput")
    m = k.dram("m", [DEPTH * 2, ADA_COLS], F32, kind="ExternalOutput")
    cs = k.sb("cs", [128, KC * 2])
    sc = k.sb("sc", [128, KC * 2])
    bs = k.sb("bs", [2, DEPTH * ADA_COLS])
    wb = [k.sb(f"wb{i}", [128, KC, 512]) for i in range(3)]
    ob = [k.sb(f"ob{i}", [2, 512]) for i in range(2)]
    ps = [k.ps(f"ps{i}", [128, 512]) for i in range(2)]
    k.dma("sp", cs[:, :], cc[:, :], cs, writes=[cs])
    for l in range(DEPTH):
        k.dma("sp", bs[:, l * ADA_COLS:(l + 1) * ADA_COLS], b[l, :].partition_broadcast(2), bs, writes=[bs.r(l)])
    k.op("act", lambda e: e.activation(out=sc[:, :], in_=cs[:, :], func=AF.Silu), reads=[cs], writes=[sc])
    it = 0
    for l in range(DEPTH):
        wl = w[l * D:(l + 1) * D, :].rearrange("(j p) n -> p j n", p=128)
        for nb in range(ADA_COLS // 512):
            wt = wb[it % 3]
            pt = ps[it % 2]
            ot = ob[it % 2]
            k.dma("sp" if it % 2 == 0 else "pool", wt[:, :, :], wl[:, :, nb * 512:(nb + 1) * 512], wt, writes=[wt])
            for j in range(KC):
                k.op("pe", lambda e, j=j, wt=wt, pt=pt: e.matmul(pt[0:2, :], sc[:, 2 * j:2 * j + 2], wt[:, j, :],
                                                             start=(j == 0), stop=(j == KC - 1)),
                     reads=[sc, wt], writes=[pt], acc=(j > 0))
            k.op("dve", lambda e, pt=pt, ot=ot, l=l, nb=nb: e.tensor_tensor(
                ot[:, :], pt[0:2, :], bs[:, l * ADA_COLS + nb * 512:l * ADA_COLS + (nb + 1) * 512], ALU.add),
                reads=[pt, bs.r(l)], writes=[ot])
            k.dma("sp", m[2 * l:2 * l + 2, nb * 512:(nb + 1) * 512], ot[:, :], ot, reads=[ot])
            it += 1
    k.emit()
    return nc


def run_ada(inp):
    cc = np.stack([inp["c"][0], inp["c_ctx"]], 0).reshape(2, KC, 128).transpose(2, 1, 0).reshape(128, KC * 2)
    cc = np.ascontiguousarray(cc)
    nc = build_ada()
    maps = []
    for c in range(NCORES):
        sl = slice(c * ADA_COLS, (c + 1) * ADA_COLS)
        maps.append({"cc": cc,
                     "w": np.ascontiguousarray(inp["w_ada"][:, :, sl]).reshape(DEPTH * D, ADA_COLS),
                     "b": np.ascontiguousarray(inp["b_ada"][:, sl])})
    res = run_bass_kernel_spmd(nc, maps, core_ids=list(range(NCORES)))
    m = np.concatenate([r["m"].reshape(DEPTH, 2, ADA_COLS) for r in res.results], axis=2)
    return m


def _copy(e, out, in_):
    if hasattr(e, "tensor_copy"):
        return e.tensor_copy(out, in_)
    return e.copy(out, in_)


class RR:
    def __init__(self, items):
        self.items = list(items)
        self.i = 0

    def __call__(self):
        v = self.items[self.i % len(self.items)]
        self.i += 1
        return v


def load_bc(k, eng, dst, src_1d):
    k.dma(eng, dst[:, :], src_1d.partition_broadcast(dst.t.shape[0]), dst, writes=[dst])


def load_w_bf16(k, wt, src, nk, ncols, stg, cast_eng, dma_eng, res=None, c0=0):
    res = res if res is not None else wt
    step = stg.items[0].t.shape[1]
    for kg in range(0, nk, step):
        kk = min(step, nk - kg)
        st = stg()
        k.dma(dma_eng(), st[:, 0:kk, 0:ncols],
              src[kg * 128:(kg + kk) * 128, :].rearrange("(j p) n -> p j n", p=128), st, writes=[st])
        ce = cast_eng()
        k.op(ce, lambda e, st=st, kg=kg, kk=kk: _copy(e, wt[:, kg:kg + kk, c0:c0 + ncols], st[:, 0:kk, 0:ncols]),
             reads=[st], writes=[res])


def rstd_from_ss(k, out_ap, in_ap, n, bufs_r, bufs_w):
    k.op("dve", lambda e: e.tensor_scalar(out_ap, in_ap, 1.0 / n, EPS, ALU.mult, ALU.add), reads=bufs_r, writes=bufs_w)
    k.op("act", lambda e: e.activation(out=out_ap, in_=out_ap, func=AF.Sqrt), reads=bufs_w, writes=bufs_w)
    k.op("dve", lambda e: e.reciprocal(out_ap, out_ap), reads=bufs_w, writes=bufs_w)


def norm_mod_T(k, xt, G1, sh1, ss, col, tmp, hb, hT_dst, tps, ident, evac):
    k.op("act", lambda e: e.activation(out=tmp[:, :], in_=xt[:, :], func=AF.Square, accum_out=ss[:, col:col + 1]),
         reads=[xt, ss.r("z")], writes=[tmp, ss.r(col)])
    rs = ss[:, col:col + 1]
    rstd_from_ss(k, rs, rs, D, [ss.r(col)], [ss.r(col)])
    k.op("dve", lambda e: e.scalar_tensor_tensor(out=tmp[:, :], in0=xt[:, :], scalar=rs, in1=G1[:, :],
                                                op0=ALU.mult, op1=ALU.mult), reads=[xt, ss.r(col), G1], writes=[tmp])
    k.op("pool", lambda e: e.tensor_tensor(tmp[:, :], tmp[:, :], sh1[:, :], ALU.add), reads=[tmp, sh1], writes=[tmp])
    k.op("act", lambda e: e.copy(hb[:, :], tmp[:, :]), reads=[tmp], writes=[hb])
    for j0 in range(0, KC, 4):
        tp = tps()
        for jj in range(4):
            j = j0 + jj
            k.op("pe", lambda e, tp=tp, jj=jj, j=j: e.transpose(tp[:, jj, :], hb[:, j * 128:(j + 1) * 128], ident[:, :]),
                 reads=[hb, ident], writes=[tp])
        dst, dres = hT_dst(j0, 4)
        k.op(evac(), lambda e, tp=tp, dst=dst: _copy(e, dst, tp[:, 0:4, :]), reads=[tp], writes=dres)


def qk_norm_rope(k, src_ap, nh, gain_bc, csb, kss, kcol, kn, t1, t2, out_bf, res_r, rope=True):
    for h in range(nh):
        k.op("act", lambda e, h=h: e.activation(out=t1[:, 0:128], in_=src_ap[:, h * 128:(h + 1) * 128], func=AF.Square,
                                                 accum_out=kss[:, kcol + h:kcol + h + 1]),
             reads=res_r + [kss.r("z")], writes=[t1, kss.r((kcol, h))])
    rsv = kss[:, kcol:kcol + nh]
    rr = [kss.r((kcol, h)) for h in range(nh)]
    rstd_from_ss(k, rsv, rsv, 128, rr, rr)
    dst = kn if rope else out_bf
    for h in range(nh):
        k.op("dve", lambda e, h=h: e.scalar_tensor_tensor(out=dst[:, h * 128:(h + 1) * 128], in0=src_ap[:, h * 128:(h + 1) * 128],
                                                         scalar=kss[:, kcol + h:kcol + h + 1], in1=gain_bc[:, :],
                                                         op0=ALU.mult, op1=ALU.mult),
             reads=res_r + [kss.r((kcol, h)), gain_bc], writes=[dst])
    if not rope:
        return [out_bf]
    n = nh * 128
    v5 = kn[:, 0:n].rearrange("p (h x y d) -> p h x y d", h=nh, x=2, y=2)
    A, B = v5[:, :, :, 0, :], v5[:, :, :, 1, :]
    o5 = out_bf[:, 0:n].rearrange("p (h x y d) -> p h x y d", h=nh, x=2, y=2)
    oA, oB = o5[:, :, :, 0, :], o5[:, :, :, 1, :]
    C = csb[:, 0:64].rearrange("p (x d) -> p x d", x=2).unsqueeze(1).broadcast_to([128, nh, 2, 32])
    S = csb[:, 64:128].rearrange("p (x d) -> p x d", x=2).unsqueeze(1).broadcast_to([128, nh, 2, 32])
    a4 = t1[:, 0:n // 2].rearrange("p (h x d) -> p h x d", h=nh, x=2)
    b4 = t2[:, 0:n // 2].rearrange("p (h x d) -> p h x d", h=nh, x=2)
    k.op("dve", lambda e: e.tensor_tensor(a4, A, C, ALU.mult), reads=[kn, csb], writes=[t1])
    k.op("pool", lambda e: e.tensor_tensor(b4, B, S, ALU.mult), reads=[kn, csb], writes=[t2])
    k.op("dve", lambda e: e.tensor_tensor(oA, a4, b4, ALU.subtract), reads=[t1, t2], writes=[out_bf.r("A")])
    k.op("dve", lambda e: e.tensor_tensor(a4, B, C, ALU.mult), reads=[kn, csb], writes=[t1])
    k.op("pool", lambda e: e.tensor_tensor(b4, A, S, ALU.mult), reads=[kn, csb], writes=[t2])
    k.op("dve", lambda e: e.tensor_tensor(oB, a4, b4, ALU.add), reads=[t1, t2], writes=[out_bf.r("B")])
    return [out_bf.r("A"), out_bf.r("B")]


NT_LAT = TPC // 128


def build_A():
    nc = new_nc()
    k = KB(nc)
    x = k.dram("x", [TPC, D], F32, kind="ExternalInput")
    vec = k.dram("vec", [3, D], F32, kind="ExternalInput")
    wkv = k.dram("wkv", [D, 1024], F32, kind="ExternalInput")
    wab = k.dram("wab", [D, 1024], F32, kind="ExternalInput")
    kg = k.dram("kg", [2, 128], F32, kind="ExternalInput")
    rope = k.dram("rope", [TPC, 128], F32, kind="ExternalInput")
    idn = k.dram("ident", [128, 128], BF16, kind="ExternalInput")
    kT_o = k.dram("kT", [128, 2, 2, TPC], BF16, kind="ExternalOutput")
    v_o = k.dram("v", [TPC, 2, 256], BF16, kind="ExternalOutput")
    u_o = k.dram("u", [2, 128, GW], F32, kind="ExternalOutput")

    ident = k.sb("ident", [128, 128], BF16)
    gb = k.sb("gb", [128, D])
    G1 = k.sb("G1", [128, D])
    sh1 = k.sb("sh1", [128, D])
    kgs = [k.sb(f"kg{i}", [128, 128]) for i in range(2)]
    ss = k.sb("ss", [128, 64])
    kss = k.sb("kss", [128, 64])
    xts = RR([k.sb(f"xt{i}", [128, D]) for i in range(2)])
    tmp = k.sb("tmp", [128, D])
    hbs = RR([k.sb(f"hb{i}", [128, D], BF16) for i in range(2)])
    hTs = RR([k.sb(f"hT{i}", [128, KC, 128], BF16) for i in range(2)])
    wts = [k.sb(f"wt{i}", [128, KC, 512], BF16) for i in range(2)]
    wa = k.sb("wa", [128, KC, 1024], BF16)
    stg = RR([k.sb(f"stg{i}", [128, 4, 512]) for i in range(2)])
    KT = k.sb("KT", [128, 2, 2, TPC], BF16)
    VS = k.sb("VS", [128, NT_LAT, 2, 256], BF16)
    css = RR([k.sb(f"cs{i}", [128, 128]) for i in range(2)])
    kn = k.sb("kn", [128, 256])
    t1 = k.sb("t1", [128, 256])
    t2 = k.sb("t2", [128, 256])
    kr = RR([k.sb(f"kr{i}", [128, 256], BF16) for i in range(2)])
    sg = k.sb("sg", [128, GW])
    us = RR([k.sb(f"us{i}", [128, GW]) for i in range(2)])
    tps = RR([k.ps(f"tp{i}", [128, 4, 128], BF16) for i in range(2)])
    pks = RR([k.ps(f"pk{i}", [128, 512]) for i in range(2)])
    pab = [k.ps(f"pab{i}", [128, 512]) for i in range(2)]
    evac = RR(["act", "dve"])
    cast = RR(["pool", "dve"])
    dq = RR(["sp", "pool"])

    k.dma("sp", ident[:, :], idn[:, :], ident, writes=[ident])
    load_bc(k, "sp", gb, vec[0, :])
    load_bc(k, "sp", G1, vec[1, :])
    load_bc(k, "sp", sh1, vec[2, :])
    for i in range(2):
        load_bc(k, "sp", kgs[i], kg[i, :])
    k.op("dve", lambda e: e.memset(ss[:, :], 0.0), writes=[ss.r("z")])
    k.op("dve", lambda e: e.memset(kss[:, :], 0.0), writes=[kss.r("z")])
    k.op("dve", lambda e: e.scalar_tensor_tensor(out=G1[:, :], in0=G1[:, :], scalar=1.0, in1=gb[:, :],
                                                op0=ALU.add, op1=ALU.mult), reads=[G1, gb], writes=[G1])
    for i in range(2):
        load_w_bf16(k, wts[i], wkv[:, i * 512:(i + 1) * 512], KC, 512, stg, cast, dq)
    for i in range(2):
        load_w_bf16(k, wa, wab[:, i * 512:(i + 1) * 512], KC, 512, stg, cast, dq, c0=i * 512)

    for t in range(NT_LAT):
        xt = xts()
        hb = hbs()
        hT = hTs()
        k.dma("sp", xt[:, :], x[t * 128:(t + 1) * 128, :], xt, writes=[xt])
        cs = css()
        k.dma("sp", cs[:, :], rope[t * 128:(t + 1) * 128, :], cs, writes=[cs])
        norm_mod_T(k, xt, G1, sh1, ss, t, tmp, hb, lambda j0, n, hT=hT: (hT[:, j0:j0 + n, :], [hT]), tps, ident, evac)
        for g in range(2):
            pk = pks()
            for j in range(KC):
                k.op("pe", lambda e, pk=pk, j=j, g=g, hT=hT: e.matmul(pk[:, :], hT[:, j, :], wts[g][:, j, :],
                                                                   start=(j == 0), stop=(j == KC - 1)),
                     reads=[hT, wts[g]], writes=[pk], acc=(j > 0))
            krb = kr()
            qk_norm_rope(k, pk[:, 0:256], 2, kgs[g], cs, kss, (t * 2 + g) * 2, kn, t1, t2, krb, [pk])
            tp = tps()
            for h in range(2):
                k.op("pe", lambda e, tp=tp, h=h, krb=krb: e.transpose(tp[:, h, :], krb[:, h * 128:(h + 1) * 128], ident[:, :]),
                     reads=[krb.r("A"), krb.r("B"), ident], writes=[tp])
            k.op(evac(), lambda e, tp=tp, g=g, t=t: _copy(e, KT[:, g, :, t * 128:(t + 1) * 128], tp[:, 0:2, :]),
                 reads=[tp], writes=[KT.r(t)])
            k.op(evac(), lambda e, pk=pk, g=g, t=t: _copy(e, VS[:, t, g, :], pk[:, 256:512]),
                 reads=[pk], writes=[VS.r(t)])
        if t in (0, NT_LAT - 1):
            for half in range(2):
                for j in range(KC):
                    k.op("pe", lambda e, j=j, half=half, hT=hT: e.matmul(pab[half][:, :], hT[:, j, :],
                                                                      wa[:, j, half * 512:(half + 1) * 512],
                                                                      start=(j == 0), stop=(j == KC - 1)),
                         reads=[hT, wa], writes=[pab[half]], acc=(j > 0))
            u = us()
            k.op("act", lambda e: e.activation(out=sg[:, :], in_=pab[1][:, :], func=AF.Sigmoid), reads=[pab[1]], writes=[sg])
            k.op("dve", lambda e, u=u: e.tensor_tensor(u[:, :], pab[0][:, :], sg[:, :], ALU.mult), reads=[pab[0], sg], writes=[u])
            k.dma("sp", u_o[0 if t == 0 else 1, :, :], u[:, :], u, reads=[u])
    allKT = [KT.r(t) for t in range(NT_LAT)]
    for g in range(2):
        k.dma(dq(), kT_o[:, g, :, :], KT[:, g, :, :], KT, reads=allKT)
    k.dma("sp", v_o[:, :, :].rearrange("(t p) g c -> p t g c", p=128), VS[:, :, :, :], VS,
          reads=[VS.r(t) for t in range(NT_LAT)])
    k.emit()
    return nc


def rope_table():
    pos = np.arange(SEQ)
    row = (pos // 64).astype(np.float32)
    col = (pos % 64).astype(np.float32)
    inv = (np.float32(10000.0) ** (-np.arange(0, 64, 2, dtype=np.float32) / np.float32(64))).astype(np.float32)
    ar = (row[:, None] * inv[None, :]).astype(np.float32)
    ac = (col[:, None] * inv[None, :]).astype(np.float32)
    return np.concatenate([np.cos(ar), np.cos(ac), np.sin(ar), np.sin(ac)], 1).astype(np.float32)


NTOK = NCTX + TPC
NTILE = NTOK // 128
V_G1, V_SC1, V_SH1, V_GT1, V_SC2, V_SH2, V_GT2 = 0, 1, 2, 3, 4, 5, 6
V_CTX = 6
V_GBR, V_G2 = 13, 14
GELU_C = 2.0 * math.sqrt(2.0 / math.pi)


def mm_acc(k, out_ap, out_res, pairs, reads):
    n = len(pairs)
    for i, (l, r) in enumerate(pairs):
        k.op("pe", lambda e, l=l, r=r, i=i: e.matmul(out_ap, l, r, start=(i == 0), stop=(i == n - 1)),
             reads=reads, writes=[out_res], acc=(i > 0))


def stage_proj(k, c):
    with k.stage():
        ident = k.sb("ident", [128, 128], BF16)
        k.dma("sp", ident[:, :], c["ident"][:, :], ident, writes=[ident])
        gb = k.sb("gb", [128, D])
        G1 = k.sb("G1", [128, D])
        sh1 = k.sb("sh1", [128, D])
        ss = k.sb("ss", [128, 32])
        xts = RR([k.sb(f"xt{i}", [128, D]) for i in range(2)])
        tmp = k.sb("tmp", [128, D])
        hbs = RR([k.sb(f"hb{i}", [128, D], BF16) for i in range(2)])
        hT = k.sb("hTall", [128, KC, NTOK], BF16)
        wts = RR([k.sb(f"wt{i}", [128, KC, 512], BF16) for i in range(2)])
        stg = RR([k.sb(f"stg{i}", [128, 4, 512]) for i in range(2)])
        pos = RR([k.sb(f"po{i}", [128, 512]) for i in range(3)])
        tps = RR([k.ps(f"tp{i}", [128, 4, 128], BF16) for i in range(2)])
        pps = RR([k.ps(f"pp{i}", [128, 512]) for i in range(4)])
        evac = RR(["act", "dve"])
        cast = RR(["pool", "dve"])
        dq = RR(["sp", "pool"])
        vec = c["vec"]
        k.op("dve", lambda e: e.memset(ss[:, :], 0.0), writes=[ss.r("z")])
        load_bc(k, "sp", gb, vec[V_G1, :])
        for t in range(NTILE):
            if t in (0, NCTX // 128):
                off = V_CTX if t == 0 else 0
                load_bc(k, "sp", G1, vec[V_SC1 + off, :])
                load_bc(k, "sp", sh1, vec[V_SH1 + off, :])
                k.op("dve", lambda e: e.scalar_tensor_tensor(out=G1[:, :], in0=G1[:, :], scalar=1.0, in1=gb[:, :],
                                                            op0=ALU.add, op1=ALU.mult), reads=[G1, gb], writes=[G1])
            xt = xts()
            k.dma(dq(), xt[:, :], c["x"][t * 128:(t + 1) * 128, :], xt, writes=[xt])
            norm_mod_T(k, xt, G1, sh1, ss, t, tmp, hbs(),
                       lambda j0, n, t=t: (hT[:, j0:j0 + n, t * 128:(t + 1) * 128], [hT.r(t)]), tps, ident, evac)
        for cb in range(IN_W // 512):
            wt = wts()
            load_w_bf16(k, wt, c["w_in"][:, cb * 512:(cb + 1) * 512], KC, 512, stg, cast, dq)
            for t in range(NTILE):
                pp = pps()
                mm_acc(k, pp[:, :], pp, [(hT[:, j, t * 128:(t + 1) * 128], wt[:, j, :]) for j in range(KC)], [hT.r(t), wt])
                po = pos()
                k.op(evac(), lambda e, po=po, pp=pp: _copy(e, po[:, :], pp[:, :]), reads=[pp], writes=[po])
                k.dma(dq(), c["P"][t * 128:(t + 1) * 128, cb * 512:(cb + 1) * 512], po[:, :], po, reads=[po])


def stage_conv(k, c):
    with k.stage():
        idf = k.sb("idf", [128, 128])
        k.dma("sp", idf[:, :], c["identf"][:, :], idf, writes=[idf])
        ones = k.sb("ones", [128, 128])
        k.op("dve", lambda e: e.memset(ones[:, :], 1.0), writes=[ones])
        cp = k.sb("cpar", [128, 136])
        k.dma("sp", cp[:, :], c["cpar"][:, :], cp, writes=[cp])
        pwb = k.sb("pwb", [128, GW])
        load_bc(k, "sp", pwb, c["pvec"][0, :])
        pw = k.sb("pw", [128, 4, GW], BF16)
        stg = RR([k.sb(f"stg{i}", [128, 4, 512]) for i in range(1)])
        load_w_bf16(k, pw, c["conv_pw"][:, :], 4, GW, stg, RR(["dve"]), RR(["sp"]))
        NL = TPC
        UT = k.sb("UT", [128, 4, NL + 2 * HALO])
        acc = k.sb("acc", [128, 4, NL])
        ctmp = k.sb("ctmp", [128, NL])
        abs_ = RR([k.sb(f"ab{i}", [128, 1024]) for i in range(2)])
        sg = k.sb("sg", [128, GW])
        us = RR([k.sb(f"u{i}", [128, GW]) for i in range(2)])
        sq = k.sb("sq", [128, 4, 512])
        mean = k.sb("mean", [128, 512])
        rstd = k.sb("rstd", [128, 512])
        zt = k.sb("zt", [128, 512])
        sT = k.sb("sT", [128, 4, 512], BF16)
        ycs = RR([k.sb(f"yc{i}", [128, GW]) for i in range(2)])
        tpf = RR([k.ps(f"tpf{i}", [128, 4, 128]) for i in range(2)])
        pst = [k.ps(f"pst{i}", [128, 512]) for i in range(2)]
        pys = RR([k.ps(f"py{i}", [128, 512]) for i in range(2)])
        dq = RR(["sp", "pool"])
        for (t0, nt, halo) in ((0, NCTX // 128, False), (NCTX // 128, NT_LAT, True)):
            N = nt * 128
            if halo:
                for side in range(2):
                    hp = us()
                    k.op("dve", lambda e, hp=hp: e.memset(hp[:, :], 0.0), writes=[hp])
                    k.dma("sp", hp[0:HALO, :], c["uh"][side, :, :], hp, writes=[hp])
                    tp = tpf()
                    for cc in range(4):
                        k.op("pe", lambda e, tp=tp, cc=cc, hp=hp: e.transpose(tp[:, cc, :], hp[:, cc * 128:(cc + 1) * 128], idf[:, :]),
                             reads=[hp, idf], writes=[tp])
                    c0 = 0 if side == 0 else HALO + N
                    k.op("act", lambda e, tp=tp, c0=c0: e.copy(UT[:, :, c0:c0 + HALO], tp[:, :, 0:HALO]),
                         reads=[tp], writes=[UT.r(("h", side))])
            else:
                k.op("dve", lambda e: e.memset(UT[:, :, 0:HALO], 0.0), writes=[UT.r(("h", 0))])
                k.op("dve", lambda e, N=N: e.memset(UT[:, :, HALO + N:2 * HALO + N], 0.0), writes=[UT.r(("h", 1))])
            for ti in range(nt):
                t = t0 + ti
                ab = abs_()
                k.dma(dq(), ab[:, :], c["P"][t * 128:(t + 1) * 128, 0:1024], ab, writes=[ab])
                k.op("act", lambda e, ab=ab: e.activation(out=sg[:, :], in_=ab[:, 512:1024], func=AF.Sigmoid), reads=[ab], writes=[sg])
                u = us()
                k.op("dve", lambda e, ab=ab, u=u: e.tensor_tensor(u[:, :], ab[:, 0:512], sg[:, :], ALU.mult), reads=[ab, sg], writes=[u])
                tp = tpf()
                for cc in range(4):
                    k.op("pe", lambda e, tp=tp, cc=cc, u=u: e.transpose(tp[:, cc, :], u[:, cc * 128:(cc + 1) * 128], idf[:, :]),
                         reads=[u, idf], writes=[tp])
                k.op("act", lambda e, tp=tp, ti=ti: e.copy(UT[:, :, HALO + ti * 128:HALO + (ti + 1) * 128], tp[:, 0:4, :]),
                     reads=[tp], writes=[UT.r(ti)])
            allU = [UT.r(ti) for ti in range(nt)] + [UT.r(("h", 0)), UT.r(("h", 1))]
            for tap in range(CONV_W):
                for cc in range(4):
                    eng = "dve" if cc < 3 else "pool"
                    w_ap = cp[:, cc * 31 + tap:cc * 31 + tap + 1]
                    if tap == 0:
                        k.op(eng, lambda e, cc=cc, w_ap=w_ap, N=N: e.tensor_scalar(
                            acc[:, cc, 0:N], UT[:, cc, 0:N], w_ap, cp[:, 124 + cc:125 + cc], ALU.mult, ALU.add),
                            reads=allU + [cp], writes=[acc.r(cc)])
                    elif eng == "dve":
                        k.op(eng, lambda e, cc=cc, w_ap=w_ap, tap=tap, N=N: e.scalar_tensor_tensor(
                            out=acc[:, cc, 0:N], in0=UT[:, cc, tap:tap + N], scalar=w_ap, in1=acc[:, cc, 0:N],
                            op0=ALU.mult, op1=ALU.add), reads=allU + [cp, acc.r(cc)], writes=[acc.r(cc)])
                    else:
                        k.op(eng, lambda e, cc=cc, w_ap=w_ap, tap=tap, N=N: e.tensor_scalar(
                            ctmp[:, 0:N], UT[:, cc, tap:tap + N], w_ap, 0.0, ALU.mult, ALU.add), reads=allU + [cp], writes=[ctmp])
                        k.op(eng, lambda e, cc=cc, N=N: e.tensor_tensor(acc[:, cc, 0:N], acc[:, cc, 0:N], ctmp[:, 0:N], ALU.add),
                             reads=[ctmp, acc.r(cc)], writes=[acc.r(cc)])
            accr = [acc.r(cc) for cc in range(4)]
            for b0 in range(0, N, 512):
                nb = min(512, N - b0)
                k.op("act", lambda e, b0=b0, nb=nb: e.activation(out=sq[:, :, 0:nb], in_=acc[:, :, b0:b0 + nb], func=AF.Square),
                     reads=accr, writes=[sq])
                mm_acc(k, pst[0][:, 0:nb], pst[0], [(ones[:, :], acc[:, cc, b0:b0 + nb]) for cc in range(4)], accr + [ones])
                mm_acc(k, pst[1][:, 0:nb], pst[1], [(ones[:, :], sq[:, cc, 0:nb]) for cc in range(4)], [sq, ones])
                k.op("act", lambda e, nb=nb: e.activation(out=mean[:, 0:nb], in_=pst[0][:, 0:nb], func=AF.Copy, scale=1.0 / GW),
                     reads=[pst[0]], writes=[mean])
                k.op("dve", lambda e, nb=nb: e.tensor_tensor(zt[:, 0:nb], mean[:, 0:nb], mean[:, 0:nb], ALU.mult), reads=[mean], writes=[zt])
                k.op("dve", lambda e, nb=nb: e.scalar_tensor_tensor(out=rstd[:, 0:nb], in0=pst[1][:, 0:nb], scalar=1.0 / GW, in1=zt[:, 0:nb],
                                                                  op0=ALU.mult, op1=ALU.subtract), reads=[pst[1], zt], writes=[rstd])
                k.op("dve", lambda e, nb=nb: e.tensor_scalar(rstd[:, 0:nb], rstd[:, 0:nb], 1.0, EPS, ALU.mult, ALU.add), reads=[rstd], writes=[rstd])
                k.op("act", lambda e, nb=nb: e.activation(out=rstd[:, 0:nb], in_=rstd[:, 0:nb], func=AF.Sqrt), reads=[rstd], writes=[rstd])
                k.op("dve", lambda e, nb=nb: e.reciprocal(rstd[:, 0:nb], rstd[:, 0:nb]), reads=[rstd], writes=[rstd])
                for cc in range(4):
                    k.op("dve", lambda e, cc=cc, b0=b0, nb=nb: e.tensor_tensor(zt[:, 0:nb], acc[:, cc, b0:b0 + nb], mean[:, 0:nb], ALU.subtract),
                         reads=[acc.r(cc), mean], writes=[zt])
                    k.op("pool", lambda e, nb=nb: e.tensor_tensor(zt[:, 0:nb], zt[:, 0:nb], rstd[:, 0:nb], ALU.mult), reads=[zt, rstd], writes=[zt])
                    k.op("dve", lambda e, cc=cc, nb=nb: e.tensor_scalar(zt[:, 0:nb], zt[:, 0:nb], cp[:, 128 + cc:129 + cc], cp[:, 132 + cc:133 + cc],
                                                                      ALU.mult, ALU.add), reads=[zt, cp], writes=[zt])
                    k.op("act", lambda e, cc=cc, nb=nb: e.activation(out=sT[:, cc, 0:nb], in_=zt[:, 0:nb], func=AF.Silu), reads=[zt], writes=[sT.r(cc)])
                sTr = [sT.r(cc) for cc in range(4)]
                for sub in range(nb // 128):
                    py = pys()
                    mm_acc(k, py[:, :], py, [(sT[:, cc, sub * 128:(sub + 1) * 128], pw[:, cc, :]) for cc in range(4)], sTr + [pw])
                    yc = ycs()
                    k.op("dve", lambda e, py=py, yc=yc: e.tensor_tensor(yc[:, :], py[:, :], pwb[:, :], ALU.add), reads=[py, pwb], writes=[yc])
                    r0 = t0 * 128 + b0 + sub * 128
                    k.dma(dq(), c["Y"][r0:r0 + 128, 0:GW], yc[:, :], yc, reads=[yc])


def stage_sgu(k, c):
    with k.stage():
        lg = k.sb("lg", [128, GW])
        lb = k.sb("lb", [128, GW])
        load_bc(k, "sp", lg, c["pvec"][1, :])
        load_bc(k, "sp", lb, c["pvec"][2, :])
        bsT = k.sb("bsT", [128, 4])
        k.dma("sp", bsT[:, :], c["bsT"][:, :], bsT, writes=[bsT])
        wsf = k.sb("wsf", [128, 4, 128])
        ws = k.sb("ws", [128, 4, 128], BF16)
        k.dma("sp", wsf[:, :, :], c["sgw"][:, :, :].rearrange("g q p -> q g p"), wsf, writes=[wsf])
        k.op("dve", lambda e: e.tensor_copy(ws[:, :, :], wsf[:, :, :]), reads=[wsf], writes=[ws])
        st = k.sb("st", [128, 64])
        k.op("dve", lambda e: e.memset(st[:, :], 0.0), writes=[st.r("z")])
        pzs = RR([k.sb(f"pz{i}", [128, 1024]) for i in range(2)])
        t1 = k.sb("t1", [128, 1024])
        zz = k.sb("zz", [128, 1024])
        vn = k.sb("vn", [128, GW])
        vbs = RR([k.sb(f"vb{i}", [128, GW], BF16) for i in range(2)])
        ys = RR([k.sb(f"ys{i}", [128, GW]) for i in range(2)])
        pss = RR([k.ps(f"ps{i}", [128, 512]) for i in range(2)])
        dq = RR(["sp", "pool"])
        for t in range(NTILE):
            pz = pzs()
            k.dma(dq(), pz[:, :], c["P"][t * 128:(t + 1) * 128, 1024:2048], pz, writes=[pz])
            k.op("pool", lambda e, pz=pz: e.tensor_tensor(t1[:, :], pz[:, :], pz[:, :], ALU.mult), reads=[pz], writes=[t1])
            k.op("dve", lambda e: e.tensor_scalar(t1[:, :], t1[:, :], 0.044715, 1.0, ALU.mult, ALU.add), reads=[t1], writes=[t1])
            k.op("pool", lambda e, pz=pz: e.tensor_tensor(t1[:, :], t1[:, :], pz[:, :], ALU.mult), reads=[t1, pz], writes=[t1])
            k.op("act", lambda e: e.activation(out=t1[:, :], in_=t1[:, :], func=AF.Sigmoid, scale=GELU_C), reads=[t1], writes=[t1])
            k.op("dve", lambda e, pz=pz: e.tensor_tensor(zz[:, :], pz[:, :], t1[:, :], ALU.mult), reads=[pz, t1], writes=[zz])
            c0 = 3 * t
            k.op("dve", lambda e, c0=c0: e.reduce_sum(out=st[:, c0:c0 + 1], in_=zz[:, 512:1024], axis=AX.X), reads=[zz, st.r("z")], writes=[st.r(c0)])
            k.op("act", lambda e, c0=c0: e.activation(out=vn[:, :], in_=zz[:, 512:1024], func=AF.Square, accum_out=st[:, c0 + 1:c0 + 2]),
                 reads=[zz, st.r("z")], writes=[vn, st.r(c0 + 1)])
            mean, ex2, var = st[:, c0:c0 + 1], st[:, c0 + 1:c0 + 2], st[:, c0 + 2:c0 + 3]
            rs = [st.r(c0), st.r(c0 + 1), st.r(c0 + 2)]
            k.op("dve", lambda e, mean=mean: e.tensor_scalar(mean, mean, 1.0 / GW, 0.0, ALU.mult, ALU.add), reads=[st.r(c0)], writes=[st.r(c0)])
            k.op("dve", lambda e, mean=mean, var=var: e.tensor_tensor(var, mean, mean, ALU.mult), reads=[st.r(c0), st.r("z")], writes=[st.r(c0 + 2)])
            k.op("dve", lambda e, ex2=ex2, var=var: e.scalar_tensor_tensor(out=var, in0=ex2, scalar=1.0 / GW, in1=var, op0=ALU.mult, op1=ALU.subtract),
                 reads=rs, writes=[st.r(c0 + 2)])
            k.op("dve", lambda e, var=var: e.tensor_scalar(var, var, 1.0, EPS, ALU.mult, ALU.add), reads=[st.r(c0 + 2)], writes=[st.r(c0 + 2)])
            k.op("act", lambda e, var=var: e.activation(out=var, in_=var, func=AF.Sqrt), reads=[st.r(c0 + 2)], writes=[st.r(c0 + 2)])
            k.op("dve", lambda e, var=var: e.reciprocal(var, var), reads=[st.r(c0 + 2)], writes=[st.r(c0 + 2)])
            k.op("dve", lambda e, mean=mean, var=var: e.tensor_scalar(vn[:, :], zz[:, 512:1024], mean, var, ALU.subtract, ALU.mult),
                 reads=[zz] + rs, writes=[vn])
            k.op("pool", lambda e: e.tensor_tensor(vn[:, :], vn[:, :], lg[:, :], ALU.mult), reads=[vn, lg], writes=[vn])
            vb = vbs()
            k.op("dve", lambda e, vb=vb: e.tensor_tensor(vb[:, :], vn[:, :], lb[:, :], ALU.add), reads=[vn, lb], writes=[vb])
            ps = pss()
            for g in range(4):
                k.op("pe", lambda e, g=g, ps=ps, vb=vb: e.matmul(ps[:, g * 128:(g + 1) * 128], ws[:, g, :], vb[:, g * 128:(g + 1) * 128],
                                                               start=True, stop=True), reads=[ws, vb], writes=[ps.r(g)])
            y = ys()
            for g in range(4):
                k.op("dve", lambda e, g=g, ps=ps, y=y: e.scalar_tensor_tensor(
                    out=y[:, g * 128:(g + 1) * 128], in0=ps[:, g * 128:(g + 1) * 128], scalar=bsT[:, g:g + 1],
                    in1=zz[:, g * 128:(g + 1) * 128], op0=ALU.add, op1=ALU.mult), reads=[ps.r(g), bsT, zz], writes=[y.r(g)])
            k.dma(dq(), c["Y"][t * 128:(t + 1) * 128, GW:2 * GW], y[:, :], y, reads=[y.r(g) for g in range(4)])


NCHUNK_G = 8


def stage_attn(k, c):
    with k.stage():
        ident = k.sb("ident", [128, 128], BF16)
        k.dma("sp", ident[:, :], c["ident"][:, :], ident, writes=[ident])
        gn = [k.sb(f"gn{i}", [128, 128]) for i in range(4)]
        for i in range(4):
            load_bc(k, "sp", gn[i], c["gains"][i, :])
        sinkb = k.sb("sinkb", [128, 4])
        load_bc(k, "sp", sinkb, c["sink"][0, :])
        mx = k.sb("mx", [128, 4])
        negb = k.sb("negb", [128, 2])
        es = k.sb("es", [128, 8])
        ga = k.sb("ga", [128, 128])
        for i in range(4):
            k.op("dve", lambda e, i=i: e.tensor_tensor(ga[:, :], gn[i][:, :], gn[i][:, :], ALU.mult), reads=[gn[i]], writes=[ga])
            k.op("dve", lambda e, i=i: e.reduce_max(out=mx[:, i:i + 1], in_=ga[:, :], axis=AX.X), reads=[ga], writes=[mx.r(i)])
        for g in range(2):
            k.op("dve", lambda e, g=g: e.tensor_tensor(negb[:, g:g + 1], mx[:, 2 * g:2 * g + 1], mx[:, 2 * g + 1:2 * g + 2], ALU.mult),
                 reads=[mx.r(2 * g), mx.r(2 * g + 1)], writes=[negb.r(g)])
            k.op("act", lambda e, g=g: e.activation(out=negb[:, g:g + 1], in_=negb[:, g:g + 1], func=AF.Sqrt), reads=[negb.r(g)], writes=[negb.r(g)])
            k.op("dve", lambda e, g=g: e.tensor_scalar(negb[:, g:g + 1], negb[:, g:g + 1], -SCALE * 128.0, 0.0, ALU.mult, ALU.add),
                 reads=[negb.r(g)], writes=[negb.r(g)])
        k.op("dve", lambda e: e.memset(es[:, :], 0.0), writes=[es])
        k.op("act", lambda e: e.activation(out=es[:, 0:4], in_=sinkb[:, :], func=AF.Exp, bias=negb[:, 0:1], scale=1.0),
             reads=[sinkb, negb.r(0), es], writes=[es])
        masks = [k.sb(f"mask{i}", [128, 128], BF16) for i in range(4)]
        for i in range(4):
            k.dma("sp", masks[i][:, :], c["masks"][i, :, :], masks[i], writes=[masks[i]])
        QT = k.sb("QT", [128, 2, 4, NTOK], BF16)
        KTc = k.sb("KTc", [128, 2, 2, NCTX], BF16)
        Vc = k.sb("Vc", [128, 2, 2, 2, 129], BF16)
        k.op("pool", lambda e: e.memset(Vc[:, :, :, :, :], 1.0), writes=[Vc])
        KTw = k.sb("KTw", [128, 2, NTOK], BF16)
        Vw = k.sb("Vw", [128, NTILE, 2, 129], BF16)
        k.dma("sp", KTw[:, :, :], c["KTs"][:, :, :], KTw, writes=[KTw])
        k.dma("pool", Vw[:, :, :, :], c["Vs"][:, :, :].rearrange("(b p) h c -> p b h c", p=128), Vw, writes=[Vw])
        kss = k.sb("kss", [128, 256])
        k.op("dve", lambda e: e.memset(kss[:, :], 0.0), writes=[kss.r("z")])
        pqs = RR([k.sb(f"pq{i}", [128, 2048]) for i in range(2)])
        css = RR([k.sb(f"cs{i}", [128, 128]) for i in range(2)])
        kn = k.sb("kn", [128, 512])
        t1 = k.sb("t1", [128, 256])
        t2 = k.sb("t2", [128, 256])
        qrs = RR([k.sb(f"qr{i}", [128, 512], BF16) for i in range(2)])
        tps = RR([k.ps(f"tp{i}", [128, 4, 128], BF16) for i in range(1)])
        evac = RR(["act", "dve"])
        dq = RR(["sp", "pool"])
        for t in range(NTILE):
            is_ctx = t < NCTX // 128
            pq = pqs()
            k.dma(dq(), pq[:, :], c["P"][t * 128:(t + 1) * 128, 2048:4096], pq, writes=[pq])
            cs = css()
            if not is_ctx:
                tl = t - NCTX // 128
                k.dma("sp", cs[:, :], c["rope"][tl * 128:(tl + 1) * 128, :], cs, writes=[cs])
            for g in range(2):
                base = g * 1024
                qr = qrs()
                rr = qk_norm_rope(k, pq[:, base:base + 512], 4, gn[2 * g], cs, kss, t * 12 + g * 6, kn, t1, t2, qr, [pq], rope=not is_ctx)
                tp = tps()
                for h in range(4):
                    k.op("pe", lambda e, tp=tp, h=h, qr=qr: e.transpose(tp[:, h, :], qr[:, h * 128:(h + 1) * 128], ident[:, :]),
                         reads=rr + [ident], writes=[tp])
                k.op(evac(), lambda e, tp=tp, g=g, t=t: _copy(e, QT[:, g, :, t * 128:(t + 1) * 128], tp[:, 0:4, :]),
                     reads=[tp], writes=[QT.r((g, t))])
                if is_ctx:
                    qr = qrs()
                    rr = qk_norm_rope(k, pq[:, base + 512:base + 768], 2, gn[2 * g + 1], cs, kss, t * 12 + g * 6 + 4, kn, t1, t2, qr, [pq], rope=False)
                    tp = tps()
                    for h in range(2):
                        k.op("pe", lambda e, tp=tp, h=h, qr=qr: e.transpose(tp[:, h, :], qr[:, h * 128:(h + 1) * 128], ident[:, :]),
                             reads=rr + [ident], writes=[tp])
                    k.op(evac(), lambda e, tp=tp, g=g, t=t: _copy(e, KTc[:, g, :, t * 128:(t + 1) * 128], tp[:, 0:2, :]),
                         reads=[tp], writes=[KTc.r((g, t))])
                    k.op("dve", lambda e, pq=pq, g=g, t=t, base=base: e.tensor_copy(
                        Vc[:, t, g, :, 0:128], pq[:, base + 768:base + 1024].rearrange("p (h d) -> p h d", h=2)),
                        reads=[pq, Vc], writes=[Vc.r((g, t))])
        sts = RR([k.ps(f"st{i}", [128, 512]) for i in range(2)])
        oacc = [k.ps(f"oa{i}", [128, 512]) for i in range(4)]
        pts = RR([k.sb(f"pt{i}", [128, 256], BF16) for i in range(3)])
        ys = RR([k.sb(f"ya{i}", [128, GW]) for i in range(2)])
        dn = k.sb("dn", [128, 8])
        KTgs = RR([k.sb(f"KTg{i}", [128, 2, 2048], BF16) for i in range(2)])
        Vgs = RR([k.sb(f"Vg{i}", [128, 16, 2, 129], BF16) for i in range(2)])
        nctx_b = NCTX // 128

        def att_block(g, t, kt_fn, v_fn, mask, first, last, reads_kv):
            for kvh in range(2):
                st = sts()
                k.op("pe", lambda e, st=st, kvh=kvh: e.matmul(st[:, 0:256], kt_fn(kvh), QT[:, g, 2 * kvh:2 * kvh + 2, t * 128:(t + 1) * 128],
                                                            start=True, stop=True), reads=reads_kv + [QT.r((g, t))], writes=[st])
                pt = pts()
                k.op("act", lambda e, st=st, pt=pt: e.activation(out=pt[:, :], in_=st[:, 0:256], func=AF.Exp, bias=negb[:, g:g + 1], scale=SCALE),
                     reads=[st, negb.r(g)], writes=[pt])
                if mask is not None:
                    k.op("dve", lambda e, pt=pt: e.tensor_tensor(pt[:, :].rearrange("p (h q) -> p h q", h=2),
                                                               pt[:, :].rearrange("p (h q) -> p h q", h=2),
                                                               mask[:, :].unsqueeze(1).broadcast_to([128, 2, 128]), ALU.mult),
                         reads=[pt, mask], writes=[pt])
                for hh in range(2):
                    oa = oacc[kvh * 2 + hh]
                    k.op("pe", lambda e, oa=oa, pt=pt, hh=hh, kvh=kvh: e.matmul(oa[:, 0:129], pt[:, hh * 128:(hh + 1) * 128], v_fn(kvh),
                                                                              start=first, stop=last),
                         reads=reads_kv + [pt], writes=[oa], acc=not first)

        def finalize(g, t):
            y = ys()
            for h in range(4):
                oa = oacc[h]
                dcol = dn[:, h:h + 1]
                k.op("dve", lambda e, oa=oa, dcol=dcol, h=h: e.tensor_scalar(dcol, oa[:, 128:129], es[:, g * 4 + h:g * 4 + h + 1], 0.0, ALU.add, ALU.add),
                     reads=[oa, es], writes=[dn.r(h)])
                k.op("dve", lambda e, dcol=dcol: e.reciprocal(dcol, dcol), reads=[dn.r(h)], writes=[dn.r(h)])
                k.op("dve", lambda e, oa=oa, dcol=dcol, h=h, y=y: e.tensor_scalar(y[:, h * 128:(h + 1) * 128], oa[:, 0:128], dcol, 0.0, ALU.mult, ALU.add),
                     reads=[oa, dn.r(h)], writes=[y.r(h)])
            k.dma(dq(), c["Y"][t * 128:(t + 1) * 128, 1024 + g * GW:1024 + (g + 1) * GW], y[:, :], y, reads=[y.r(h) for h in range(4)])

        ctx_res = lambda g: [KTc.r((g, b)) for b in range(nctx_b)] + [Vc.r((g, b)) for b in range(nctx_b)]
        for t in range(NTILE):
            is_ctx = t < nctx_b
            i = t - nctx_b
            blocks = [("c", b, None) for b in range(nctx_b)]
            if not is_ctx:
                blocks += [("w", i, masks[2] if i == 0 else masks[0]), ("w", i + 1, None),
                           ("w", i + 2, masks[3] if i == NT_LAT - 1 else masks[1])]
            for bi, (kind, b, mask) in enumerate(blocks):
                if kind == "c":
                    att_block(0, t, lambda kvh, b=b: KTc[:, 0, kvh, b * 128:(b + 1) * 128], lambda kvh, b=b: Vc[:, b, 0, kvh, :],
                              mask, bi == 0, bi == len(blocks) - 1, ctx_res(0))
                else:
                    att_block(0, t, lambda kvh, b=b: KTw[:, kvh, b * 128:(b + 1) * 128], lambda kvh, b=b: Vw[:, b, kvh, :],
                              mask, bi == 0, bi == len(blocks) - 1, [KTw, Vw])
            finalize(0, t)
            nblk = nctx_b + (0 if is_ctx else SEQ // 128)
            for b in range(nctx_b):
                att_block(1, t, lambda kvh, b=b: KTc[:, 1, kvh, b * 128:(b + 1) * 128], lambda kvh, b=b: Vc[:, b, 1, kvh, :],
                          None, b == 0, b == nblk - 1, ctx_res(1))
            if not is_ctx:
                for ci in range(NCHUNK_G):
                    KTg, Vg = KTgs(), Vgs()
                    k.dma("sp", KTg[:, :, :], c["KTg"][:, :, ci * 2048:(ci + 1) * 2048], KTg, writes=[KTg])
                    k.dma("pool", Vg[:, :, :, :], c["Vg"][ci * 2048:(ci + 1) * 2048, :, :].rearrange("(b p) h c -> p b h c", p=128), Vg, writes=[Vg])
                    for bb in range(16):
                        bidx = nctx_b + ci * 16 + bb
                        att_block(1, t, lambda kvh, bb=bb, KTg=KTg: KTg[:, kvh, bb * 128:(bb + 1) * 128],
                                  lambda kvh, bb=bb, Vg=Vg: Vg[:, bb, kvh, :], None, False, bidx == nblk - 1, [KTg, Vg])
            finalize(1, t)


def router_tile(k, L, plg, brb, c, t):
    lg, m1, m2, mk1, mk2, l2, ed, g1, g2, gate = (L[:, 0:8], L[:, 8:9], L[:, 9:10], L[:, 16:24], L[:, 24:32], L[:, 32:40],
                                                 L[:, 10:11], L[:, 11:12], L[:, 12:13], L[:, 40:48])
    ops = [
        lambda e: e.tensor_tensor(lg, plg[:, 0:NEXP], brb[:, :], ALU.add),
        lambda e: e.reduce_max(out=m1, in_=lg, axis=AX.X),
        lambda e: e.tensor_scalar(mk1, lg, m1, 0.0, ALU.is_equal, ALU.add),
        lambda e: e.scalar_tensor_tensor(out=l2, in0=mk1, scalar=-1e30, in1=lg, op0=ALU.mult, op1=ALU.add),
        lambda e: e.reduce_max(out=m2, in_=l2, axis=AX.X),
        lambda e: e.tensor_scalar(mk2, l2, m2, 0.0, ALU.is_equal, ALU.add),
        lambda e: e.tensor_tensor(ed, m2, m1, ALU.subtract),
    ]
    for f in ops:
        k.op("dve", f, reads=[plg, brb, L], writes=[L])
    k.op("act", lambda e: e.activation(out=ed, in_=ed, func=AF.Exp), reads=[L], writes=[L])
    ops = [
        lambda e: e.tensor_scalar(g1, ed, 1.0, 0.0, ALU.add, ALU.add),
        lambda e: e.reciprocal(g1, g1),
        lambda e: e.tensor_tensor(g2, ed, g1, ALU.mult),
        lambda e: e.tensor_scalar(gate, mk1, g1, 0.0, ALU.mult, ALU.add),
        lambda e: e.scalar_tensor_tensor(out=gate, in0=mk2, scalar=g2, in1=gate, op0=ALU.mult, op1=ALU.add),
    ]
    for f in ops:
        k.op("dve", f, reads=[L], writes=[L])
    k.dma("sp", c["gates"][t * 128:(t + 1) * 128, :], gate, L, reads=[L])


def stage_out(k, c, moe):
    with k.stage():
        ident = k.sb("ident", [128, 128], BF16)
        k.dma("sp", ident[:, :], c["ident"][:, :], ident, writes=[ident])
        vec = c["vec"]
        wo = k.sb("wo", [128, KC, D], BF16)
        stg = RR([k.sb(f"stg{i}", [128, 4, 512]) for i in range(2)])
        cast = RR(["pool", "dve"])
        dq = RR(["sp", "pool"])
        for cb in range(4):
            load_w_bf16(k, wo, c["w_out"][:, cb * 512:(cb + 1) * 512], KC, 512, stg, cast, dq, res=wo.r(cb), c0=cb * 512)
        gbr = k.sb("gbr", [128, D])
        load_bc(k, "sp", gbr, vec[V_GBR, :])
        g2b = k.sb("g2b", [128, D])
        load_bc(k, "sp", g2b, vec[V_G2, :])
        gt1 = k.sb("gt1", [128, D])
        G2 = k.sb("G2", [128, D])
        sh2 = k.sb("sh2", [128, D])
        gs = k.sb("gs", [128, 128])
        ss2 = k.sb("ss2", [128, 32])
        k.op("dve", lambda e: e.memset(gs[:, :], 0.0), writes=[gs.r("z")])
        k.op("dve", lambda e: e.memset(ss2[:, :], 0.0), writes=[ss2.r("z")])
        xt = k.sb("xt", [128, D])
        yt = k.sb("yt", [128, D])
        x1 = k.sb("x1", [128, D])
        tmp = k.sb("tmp", [128, D])
        tmc = k.sb("tmc", [128, 512])
        hb = k.sb("hb", [128, D], BF16)
        yTs = RR([k.sb(f"yT{i}", [128, KC, 128], BF16) for i in range(2)])
        h2s = RR([k.sb(f"h2{i}", [128, KC, 128], BF16) for i in range(2)])
        tps = RR([k.ps(f"tp{i}", [128, 4, 128], BF16) for i in range(2)])
        pps = RR([k.ps(f"pp{i}", [128, 512]) for i in range(3)])
        evac = RR(["act", "dve"])
        if moe:
            idf = k.sb("idf", [128, 128])
            k.dma("sp", idf[:, :], c["identf"][:, :], idf, writes=[idf])
            wr = k.sb("wr", [128, KC, NEXP])
            k.dma("sp", wr[:, :, :], c["router_w"][:, :].rearrange("(j p) n -> p j n", p=128), wr, writes=[wr])
            brb = k.sb("brb", [128, NEXP])
            load_bc(k, "sp", brb, c["router_b"][0, :])
            hfT = k.sb("hfT", [128, KC, 128])
            tpf = RR([k.ps(f"tpf{i}", [128, 4, 128]) for i in range(1)])
            plg = k.ps("plg", [128, 512])
            lgs = RR([k.sb(f"lg{i}", [128, 64]) for i in range(2)])
        for t in range(NTILE):
            if t in (0, NCTX // 128):
                off = V_CTX if t == 0 else 0
                load_bc(k, "sp", gt1, vec[V_GT1 + off, :])
                load_bc(k, "sp", G2, vec[V_SC2 + off, :])
                load_bc(k, "sp", sh2, vec[V_SH2 + off, :])
                k.op("dve", lambda e: e.scalar_tensor_tensor(out=G2[:, :], in0=G2[:, :], scalar=1.0, in1=g2b[:, :],
                                                            op0=ALU.add, op1=ALU.mult), reads=[G2, g2b], writes=[G2])
            k.dma("sp", yt[:, :], c["Y"][t * 128:(t + 1) * 128, :], yt, writes=[yt])
            k.dma("pool", xt[:, :], c["x"][t * 128:(t + 1) * 128, :], xt, writes=[xt])
            for gi in range(4):
                k.op("act", lambda e, gi=gi, t=t: e.activation(out=tmp[:, 0:GW], in_=yt[:, gi * GW:(gi + 1) * GW], func=AF.Square,
                                                              accum_out=gs[:, 4 * t + gi:4 * t + gi + 1]),
                     reads=[yt, gs.r("z")], writes=[tmp, gs.r((t, gi))])
            rr = [gs.r((t, gi)) for gi in range(4)]
            rstd_from_ss(k, gs[:, 4 * t:4 * t + 4], gs[:, 4 * t:4 * t + 4], GW, rr, rr)
            for gi in range(4):
                k.op("dve", lambda e, gi=gi, t=t: e.scalar_tensor_tensor(
                    out=hb[:, gi * GW:(gi + 1) * GW], in0=yt[:, gi * GW:(gi + 1) * GW], scalar=gs[:, 4 * t + gi:4 * t + gi + 1],
                    in1=gbr[:, gi * GW:(gi + 1) * GW], op0=ALU.mult, op1=ALU.mult), reads=[yt, gs.r((t, gi)), gbr], writes=[hb])
            yT = yTs()
            for j0 in range(0, KC, 4):
                tp = tps()
                for jj in range(4):
                    j = j0 + jj
                    k.op("pe", lambda e, tp=tp, jj=jj, j=j: e.transpose(tp[:, jj, :], hb[:, j * 128:(j + 1) * 128], ident[:, :]),
                         reads=[hb, ident], writes=[tp])
                k.op(evac(), lambda e, tp=tp, j0=j0, yT=yT: _copy(e, yT[:, j0:j0 + 4, :], tp[:, 0:4, :]), reads=[tp], writes=[yT])
            for cb in range(4):
                pp = pps()
                mm_acc(k, pp[:, :], pp, [(yT[:, j, :], wo[:, j, cb * 512:(cb + 1) * 512]) for j in range(KC)], [yT, wo.r(cb)])
                k.op("dve", lambda e, pp=pp, cb=cb: e.tensor_tensor(tmc[:, :], pp[:, :], gt1[:, cb * 512:(cb + 1) * 512], ALU.mult),
                     reads=[pp, gt1], writes=[tmc])
                k.op("pool", lambda e, cb=cb: e.tensor_tensor(x1[:, cb * 512:(cb + 1) * 512], tmc[:, :], xt[:, cb * 512:(cb + 1) * 512], ALU.add),
                     reads=[tmc, xt], writes=[x1])
            k.dma("sp", c["X1"][t * 128:(t + 1) * 128, :], x1[:, :], x1, reads=[x1])
            h2 = h2s()
            norm_mod_T(k, x1, G2, sh2, ss2, t, tmp, hb, lambda j0, n, h2=h2: (h2[:, j0:j0 + n, :], [h2]), tps, ident, evac)
            k.dma("pool", c["h2T"][:, :, t * 128:(t + 1) * 128], h2[:, :, :], h2, reads=[h2])
            if moe:
                for j0 in range(0, KC, 4):
                    tp = tpf()
                    for jj in range(4):
                        j = j0 + jj
                        k.op("pe", lambda e, tp=tp, jj=jj, j=j: e.transpose(tp[:, jj, :], tmp[:, j * 128:(j + 1) * 128], idf[:, :]),
                             reads=[tmp, idf], writes=[tp])
                    k.op(evac(), lambda e, tp=tp, j0=j0: _copy(e, hfT[:, j0:j0 + 4, :], tp[:, 0:4, :]), reads=[tp], writes=[hfT])
                mm_acc(k, plg[:, 0:NEXP], plg, [(hfT[:, j, :], wr[:, j, :]) for j in range(KC)], [hfT, wr])
                router_tile(k, lgs(), plg, brb, c, t)


def stage_ffn(k, c, ntok, expert):
    with k.stage():
        G = 512
        hid = k.sb("hid", [128, FC, G], BF16)
        h2g = RR([k.sb(f"h2g{i}", [128, KC, G], BF16) for i in range(2)])
        w1s = RR([k.sb(f"w1b{i}", [128, KC, 256], BF16) for i in range(2)])
        w3s = RR([k.sb(f"w3b{i}", [128, KC, 256], BF16) for i in range(2)])
        w2s = RR([k.sb(f"w2b{i}", [128, 4, 512], BF16) for i in range(3)])
        stg = RR([k.sb(f"stg{i}", [128, 4, 512]) for i in range(3)])
        sas = RR([k.sb(f"sa{i}", [128, G]) for i in range(2)])
        obs = RR([k.sb(f"ob{i}", [128, 512]) for i in range(3)])
        pas = RR([k.ps(f"pa{i}", [128, 512]) for i in range(2)])
        pbs = RR([k.ps(f"pb{i}", [128, 512]) for i in range(2)])
        pos = [k.ps(f"po{i}", [128, 512]) for i in range(4)]
        cast = RR(["pool", "dve", "pool", "act"])
        dq = RR(["sp", "pool"])
        ntile = ntok // 128
        if expert:
            gcol = k.sb("gcol", [128, ntile])
            k.dma("sp", gcol[:, :], c["gcol"][:, :], gcol, writes=[gcol])
        else:
            gt2 = [k.sb(f"gt2{i}", [128, D]) for i in range(2)]
            load_bc(k, "sp", gt2[0], c["vec"][V_GT2 + V_CTX, :])
            load_bc(k, "sp", gt2[1], c["vec"][V_GT2, :])
            x1s = RR([k.sb(f"x1c{i}", [128, 512]) for i in range(3)])
        for g0 in range(0, ntok, G):
            gsz = min(G, ntok - g0)
            hg = h2g()
            k.dma("sp", hg[:, :, 0:gsz], c["h2T"][:, :, g0:g0 + gsz], hg, writes=[hg])
            for fb in range(FC // 2):
                w1b, w3b = w1s(), w3s()
                load_w_bf16(k, w1b, c["w1"][:, fb * 256:(fb + 1) * 256], KC, 256, stg, cast, dq)
                load_w_bf16(k, w3b, c["w3"][:, fb * 256:(fb + 1) * 256], KC, 256, stg, cast, dq)
                for jj in range(2):
                    j = fb * 2 + jj
                    pa, pb = pas(), pbs()
                    mm_acc(k, pa[:, 0:gsz], pa, [(w1b[:, kc, jj * 128:(jj + 1) * 128], hg[:, kc, 0:gsz]) for kc in range(KC)], [w1b, hg])
                    mm_acc(k, pb[:, 0:gsz], pb, [(w3b[:, kc, jj * 128:(jj + 1) * 128], hg[:, kc, 0:gsz]) for kc in range(KC)], [w3b, hg])
                    sa = sas()
                    k.op("act", lambda e, sa=sa, pa=pa, gsz=gsz: e.activation(out=sa[:, 0:gsz], in_=pa[:, 0:gsz], func=AF.Silu), reads=[pa], writes=[sa])
                    k.op("dve", lambda e, sa=sa, pb=pb, j=j, gsz=gsz: e.tensor_tensor(hid[:, j, 0:gsz], sa[:, 0:gsz], pb[:, 0:gsz], ALU.mult),
                         reads=[sa, pb], writes=[hid.r(j)])
            nsub = gsz // 128
            for cb in range(4):
                for jb in range(FC // 4):
                    w2b = w2s()
                    load_w_bf16(k, w2b, c["w2"][jb * 512:(jb + 1) * 512, cb * 512:(cb + 1) * 512], 4, 512, stg, cast, dq)
                    for sub in range(nsub):
                        for q in range(4):
                            j = jb * 4 + q
                            first = (jb == 0 and q == 0)
                            last = (jb == FC // 4 - 1 and q == 3)
                            k.op("pe", lambda e, sub=sub, j=j, q=q, w2b=w2b, first=first, last=last: e.matmul(
                                pos[sub][:, :], hid[:, j, sub * 128:(sub + 1) * 128], w2b[:, q, :], start=first, stop=last),
                                reads=[hid.r(j), w2b], writes=[pos[sub]], acc=not first)
                for sub in range(nsub):
                    tile = (g0 + sub * 128) // 128
                    r0 = g0 + sub * 128
                    ob = obs()
                    if expert:
                        k.op("act", lambda e, ob=ob, sub=sub, tile=tile: e.activation(out=ob[:, :], in_=pos[sub][:, :], func=AF.Copy,
                                                                                   scale=gcol[:, tile:tile + 1]), reads=[pos[sub], gcol], writes=[ob])
                    else:
                        gv = gt2[0] if tile < NCTX // 128 else gt2[1]
                        xc = x1s()
                        k.dma("sp", xc[:, :], c["X1"][r0:r0 + 128, cb * 512:(cb + 1) * 512], xc, writes=[xc])
                        k.op("dve", lambda e, ob=ob, sub=sub, gv=gv, cb=cb: e.tensor_tensor(ob[:, :], pos[sub][:, :], gv[:, cb * 512:(cb + 1) * 512], ALU.mult),
                             reads=[pos[sub], gv], writes=[ob])
                        k.op("pool", lambda e, ob=ob, xc=xc: e.tensor_tensor(ob[:, :], ob[:, :], xc[:, :], ALU.add), reads=[ob, xc], writes=[ob])
                    k.dma(dq(), c["out"][r0:r0 + 128, cb * 512:(cb + 1) * 512], ob[:, :], ob, reads=[ob])


B_INPUTS = [("x", [NTOK, D], F32), ("vec", [15, D], F32), ("w_in", [D, IN_W], F32), ("w_out", [D, D], F32),
            ("conv_pw", [GW, GW], F32), ("cpar", [128, 136], F32), ("pvec", [3, GW], F32), ("sgw", [4, 128, 128], F32),
            ("bsT", [128, 4], F32), ("gains", [4, 128], F32), ("sink", [1, 4], F32), ("rope", [TPC, 128], F32),
            ("KTs", [128, 2, NTOK], BF16), ("Vs", [NTOK, 2, 129], BF16), ("KTg", [128, 2, SEQ], BF16), ("Vg", [SEQ, 2, 129], BF16),
            ("uh", [2, HALO, GW], F32), ("masks", [4, 128, 128], BF16), ("ident", [128, 128], BF16), ("identf", [128, 128], F32)]


def build_B(moe, dbg=False):
    nc = new_nc()
    k = KB(nc)
    c = {}
    for name, shape, dt in B_INPUTS:
        c[name] = k.dram(name, shape, dt, kind="ExternalInput")
    kind_dbg = "ExternalOutput" if dbg else "Internal"
    c["P"] = k.dram("P", [NTOK, IN_W], F32, kind=kind_dbg)
    c["Y"] = k.dram("Y", [NTOK, D], F32, kind=kind_dbg)
    if moe:
        c["router_w"] = k.dram("router_w", [D, NEXP], F32, kind="ExternalInput")
        c["router_b"] = k.dram("router_b", [1, NEXP], F32, kind="ExternalInput")
        c["X1"] = k.dram("X1", [NTOK, D], F32, kind="ExternalOutput")
        c["h2T"] = k.dram("h2T", [128, KC, NTOK], BF16, kind="ExternalOutput")
        c["gates"] = k.dram("gates", [NTOK, NEXP], F32, kind="ExternalOutput")
    else:
        for nm, shp in (("w1", [D, DFF]), ("w3", [D, DFF]), ("w2", [DFF, D])):
            c[nm] = k.dram(nm, shp, F32, kind="ExternalInput")
        c["X1"] = k.dram("X1", [NTOK, D], F32, kind=kind_dbg)
        c["h2T"] = k.dram("h2T", [128, KC, NTOK], BF16, kind=kind_dbg)
        c["out"] = k.dram("out", [NTOK, D], F32, kind="ExternalOutput")
    stage_proj(k, c)
    stage_conv(k, c)
    stage_sgu(k, c)
    stage_attn(k, c)
    stage_out(k, c, moe)
    if not moe:
        stage_ffn(k, c, NTOK, expert=False)
    k.emit()
    return nc


NALL = NCTX + SEQ


def build_E(ntok=NALL):
    nc = new_nc()
    k = KB(nc)
    c = {"h2T": k.dram("h2T", [128, KC, ntok], BF16, kind="ExternalInput"),
         "gcol": k.dram("gcol", [128, ntok // 128], F32, kind="ExternalInput"),
         "w1": k.dram("w1", [D, DFF], F32, kind="ExternalInput"),
         "w3": k.dram("w3", [D, DFF], F32, kind="ExternalInput"),
         "w2": k.dram("w2", [DFF, D], F32, kind="ExternalInput"),
         "out": k.dram("out", [ntok, D], F32, kind="ExternalOutput")}
    stage_ffn(k, c, ntok, expert=True)
    k.emit()
    return nc


def build_C(nparts=NEXP):
    nc = new_nc()
    k = KB(nc)
    X1 = k.dram("X1", [NTOK, D], F32, kind="ExternalInput")
    part = k.dram("part", [nparts * NTOK, D], F32, kind="ExternalInput")
    vec2 = k.dram("vec2", [2, D], F32, kind="ExternalInput")
    out = k.dram("out", [NTOK, D], F32, kind="ExternalOutput")
    gt2 = [k.sb(f"gt2{i}", [128, D]) for i in range(2)]
    for i in range(2):
        load_bc(k, "sp", gt2[i], vec2[i, :])
    pts = RR([k.sb(f"pt{i}", [128, D]) for i in range(10)])
    xts = RR([k.sb(f"xt{i}", [128, D]) for i in range(2)])
    dq = RR(["sp", "pool"])
    eng = RR(["dve", "pool"])
    for t in range(NTILE):
        xt = xts()
        k.dma(dq(), xt[:, :], X1[t * 128:(t + 1) * 128, :], xt, writes=[xt])
        ps_ = []
        for e_ in range(nparts):
            p = pts()
            k.dma(dq(), p[:, :], part[e_ * NTOK + t * 128:e_ * NTOK + (t + 1) * 128, :], p, writes=[p])
            ps_.append(p)
        for a, b in (((0, 1), (2, 3), (4, 5), (6, 7), (0, 2), (4, 6), (0, 4)) if nparts == 8 else ((0, 1),)):
            k.op(eng(), lambda e, a=ps_[a], b=ps_[b]: e.tensor_tensor(a[:, :], a[:, :], b[:, :], ALU.add), reads=[ps_[a], ps_[b]], writes=[ps_[a]])
        gv = gt2[0] if t < NCTX // 128 else gt2[1]
        s0 = ps_[0]
        k.op("dve", lambda e, s0=s0, gv=gv: e.tensor_tensor(s0[:, :], s0[:, :], gv[:, :], ALU.mult), reads=[s0, gv], writes=[s0])
        k.op("pool", lambda e, s0=s0, xt=xt: e.tensor_tensor(s0[:, :], s0[:, :], xt[:, :], ALU.add), reads=[s0, xt], writes=[s0])
        k.dma(dq(), out[t * 128:(t + 1) * 128, :], s0[:, :], s0, reads=[s0])
    k.emit()
    return nc


_NC_CACHE = {}


def _get_nc(name, fn):
    if name not in _NC_CACHE:
        _NC_CACHE[name] = fn()
    return _NC_CACHE[name]


def _run(name, fn, maps):
    nc = _get_nc(name, fn)
    return run_bass_kernel_spmd(nc, maps, core_ids=list(range(NCORES))).results


def _c(a):
    return np.ascontiguousarray(a)


def kernel(**inp):
    f32 = np.float32
    inp = {k_: np.asarray(v) for k_, v in inp.items()}
    m = run_ada(inp)
    x_lat = _c(inp["x"][0]).astype(f32, copy=False)
    x_ctx = _c(inp["ctx"][0]).astype(f32, copy=False)
    rope = rope_table()
    ident = np.eye(128, dtype=f32).astype(NPBF)
    identf = np.eye(128, dtype=f32)
    kq = np.arange(128)
    tri_prev = (kq[:, None] >= kq[None, :]).astype(f32)
    tri_next = (kq[:, None] <= kq[None, :]).astype(f32)
    zero = np.zeros((128, 128), f32)
    for l in range(DEPTH):
        moe = (l % 2 == 1)
        sh1, sc1, gt1, sh2, sc2, gt2 = m[l, 0].reshape(6, D)
        csh1, csc1, cgt1, csh2, csc2, cgt2 = m[l, 1].reshape(6, D)
        vec = _c(np.stack([inp["g_norm1"][l], sc1, sh1, gt1, sc2, sh2, gt2, csc1, csh1, cgt1, csc2, csh2, cgt2,
                           inp["g_branch"][l], inp["g_norm2"][l]]).astype(f32))
        w_in = inp["w_in"][l]
        wkv = _c(np.concatenate([w_in[:, 2560:3072], w_in[:, 3584:4096]], axis=1))
        wab = _c(w_in[:, 0:1024])
        kg = _c(np.stack([inp["swa_k_g"][l], inp["glb_k_g"][l]]))
        maps = [{"x": x_lat[c * TPC:(c + 1) * TPC], "vec": _c(vec[0:3]), "wkv": wkv, "wab": wab, "kg": kg,
                 "rope": rope[c * TPC:(c + 1) * TPC], "ident": ident} for c in range(NCORES)]
        ra = _run("A", build_A, maps)
        KTg = _c(np.concatenate([ra[c]["kT"][:, 1] for c in range(NCORES)], axis=2))
        Vg = np.ones((SEQ, 2, 129), NPBF)
        Vg[:, :, :128] = np.concatenate([ra[c]["v"][:, 1].reshape(TPC, 2, 128) for c in range(NCORES)], axis=0)
        maps = []
        for c in range(NCORES):
            KTs = np.zeros((128, 2, NTOK), NPBF)
            Vs = np.ones((NTOK, 2, 129), NPBF)
            Vs[:, :, :128] = 0
            uh = np.zeros((2, HALO, GW), f32)
            KTs[:, :, 128:128 + TPC] = ra[c]["kT"][:, 0]
            Vs[128:128 + TPC, :, :128] = ra[c]["v"][:, 0].reshape(TPC, 2, 128)
            if c > 0:
                KTs[:, :, 0:128] = ra[c - 1]["kT"][:, 0][:, :, TPC - 128:]
                Vs[0:128, :, :128] = ra[c - 1]["v"][TPC - 128:, 0].reshape(128, 2, 128)
                uh[0] = ra[c - 1]["u"][1][128 - HALO:]
            if c < NCORES - 1:
                KTs[:, :, 128 + TPC:] = ra[c + 1]["kT"][:, 0][:, :, 0:128]
                Vs[128 + TPC:, :, :128] = ra[c + 1]["v"][0:128, 0].reshape(128, 2, 128)
                uh[1] = ra[c + 1]["u"][0][0:HALO]
            masks = np.stack([tri_prev, tri_next, zero if c == 0 else tri_prev, zero if c == NCORES - 1 else tri_next]).astype(NPBF)
            cw = inp["conv_w"][l]
            cpar = np.concatenate([cw.T.reshape(4, 128, CONV_W).transpose(1, 0, 2).reshape(128, 4 * CONV_W),
                                   inp["conv_b"][l].reshape(4, 128).T, inp["conv_ln_g"][l].reshape(4, 128).T,
                                   inp["conv_ln_b"][l].reshape(4, 128).T], axis=1)
            d = {"x": _c(np.concatenate([x_ctx, x_lat[c * TPC:(c + 1) * TPC]], axis=0)), "vec": vec, "w_in": w_in,
                 "w_out": inp["w_out"][l], "conv_pw": inp["conv_pw"][l], "cpar": _c(cpar.astype(f32)),
                 "pvec": _c(np.stack([inp["conv_pw_b"][l], inp["sgu_ln_g"][l], inp["sgu_ln_b"][l]])),
                 "sgw": _c(inp["sgu_w"][l].transpose(0, 2, 1)), "bsT": _c(inp["sgu_b"][l].T),
                 "gains": _c(np.stack([inp["swa_q_g"][l], inp["swa_k_g"][l], inp["glb_q_g"][l], inp["glb_k_g"][l]])),
                 "sink": _c(inp["swa_sink"][l][None, :]), "rope": rope[c * TPC:(c + 1) * TPC],
                 "KTs": KTs, "Vs": Vs, "KTg": KTg, "Vg": Vg, "uh": uh, "masks": masks, "ident": ident, "identf": identf}
            if moe:
                d["router_w"] = inp["router_w"][l // 2]
                d["router_b"] = _c(inp["router_b"][l // 2][None, :])
            else:
                d["w1"], d["w3"], d["w2"] = inp["ffn_w1"][l // 2], inp["ffn_w3"][l // 2], inp["ffn_w2"][l // 2]
            maps.append(d)
        del ra
        if not moe:
            rb = _run("Bd", lambda: build_B(False), maps)
            del maps
            x_ctx = _c(rb[0]["out"][:NCTX])
            x_lat = _c(np.concatenate([rb[c]["out"][NCTX:] for c in range(NCORES)], axis=0))
            del rb
            continue
        rb = _run("Bm", lambda: build_B(True), maps)
        del maps
        h2T = _c(np.concatenate([rb[0]["h2T"][:, :, :NCTX]] + [rb[c]["h2T"][:, :, NCTX:] for c in range(NCORES)], axis=2))
        gates = np.concatenate([rb[0]["gates"][:NCTX]] + [rb[c]["gates"][NCTX:] for c in range(NCORES)], axis=0)
        X1 = [rb[c]["X1"] for c in range(NCORES)]
        del rb
        sel = gates > 0
        idxs = [np.nonzero(sel[:, e])[0] for e in range(NEXP)]
        cap = max(128, -(-max(len(ix) for ix in idxs) // 128) * 128)
        maps = []
        for e in range(NEXP):
            n_e = len(idxs[e])
            h_e = np.zeros((128, KC, cap), NPBF)
            h_e[:, :, :n_e] = h2T[:, :, idxs[e]]
            g_e = np.zeros((cap,), f32)
            g_e[:n_e] = gates[idxs[e], e]
            maps.append({"h2T": h_e, "gcol": _c(g_e.reshape(cap // 128, 128).T),
                         "w1": inp["exp_w1"][l // 2][e], "w3": inp["exp_w3"][l // 2][e], "w2": inp["exp_w2"][l // 2][e]})
        re_ = _run(("E", cap), lambda: build_E(cap), maps)
        del maps, h2T
        outs = [re_[e]["out"] for e in range(NEXP)]
        del re_
        order = np.argsort(~sel, axis=1, kind="stable")[:, :2]
        parts_all = np.zeros((2, NALL, D), f32)
        for e in range(NEXP):
            inv = np.full((NALL,), -1, np.int64)
            inv[idxs[e]] = np.arange(len(idxs[e]))
            for s_ in range(2):
                tok = np.nonzero((order[:, s_] == e) & sel[:, e])[0]
                parts_all[s_, tok] = outs[e][inv[tok]]
        vec2 = _c(np.stack([cgt2, gt2]).astype(f32))
        maps = []
        for c in range(NCORES):
            part = np.concatenate([np.concatenate([parts_all[s_, :NCTX], parts_all[s_, NCTX + c * TPC:NCTX + (c + 1) * TPC]], axis=0)
                                   for s_ in range(2)], axis=0)
            maps.append({"X1": X1[c], "part": part, "vec2": vec2})
        del parts_all
        del outs
        rc = _run("C2", lambda: build_C(2), maps)
        del maps
        x_ctx = _c(rc[0]["out"][:NCTX])
        x_lat = _c(np.concatenate([rc[c]["out"][NCTX:] for c in range(NCORES)], axis=0))
        del rc
    return x_lat[None].astype(f32)
```

```python
import contextlib
import math
import numpy as np
import ml_dtypes
import concourse.bass as bass
import concourse.mybir as mybir
from concourse.bass_utils import run_bass_kernel_spmd

F32 = mybir.dt.float32
BF16 = mybir.dt.bfloat16
AF = mybir.ActivationFunctionType
ALU = mybir.AluOpType
AX = mybir.AxisListType
NPBF = ml_dtypes.bfloat16

NCORES = 8
D = 2048
KC = D // 128
SEQ = 16384
TPC = SEQ // NCORES
NCTX = 256
DEPTH = 4
GW = 512
IN_W = 4096
DFF = 5632
FC = DFF // 128
NEXP = 8
EPS = 1e-6
CONV_W = 31
HALO = 15
SCALE = 128 ** -0.5


class Res:
    __slots__ = ("last_w", "readers")

    def __init__(self):
        self.last_w = None
        self.readers = []


class Buf(Res):
    def __init__(self, t, name):
        super().__init__()
        self.t = t
        self.name = name
        self.sub = {}
        self.sem = None
        self.dma_total = 0
        self.psum = False

    def r(self, key):
        if self.psum:
            return self
        s = self.sub.get(key)
        if s is None:
            s = self.sub[key] = Res()
        return s

    def __getitem__(self, key):
        return self.t[key]


class Op:
    __slots__ = ("eng", "fn", "deps", "signal", "val", "is_dma", "dsem", "dval")

    def __init__(self, eng, fn):
        self.eng = eng
        self.fn = fn
        self.deps = []
        self.signal = False
        self.val = 0
        self.is_dma = False
        self.dsem = None
        self.dval = 0


ENGS = ("pe", "act", "dve", "pool", "sp")


class KB:
    def __init__(self, nc):
        self.nc = nc
        self.es = contextlib.ExitStack()
        self.ops = {e: [] for e in ENGS}
        self.sems = {}
        self.dma_bufs = []
        self.nbuf = 0
        self.pending = {}
        self.sem_pool = []
        self.stack = self.es
        self.stage_bufs = None

    def sem(self, name):
        return self.es.enter_context(self.nc.semaphore(name))

    def sb(self, name, shape, dtype=F32):
        self.nbuf += 1
        b = Buf(self.stack.enter_context(self.nc.sbuf_tensor(f"{name}_{self.nbuf}", list(shape), dtype)), name)
        if self.stage_bufs is not None:
            self.stage_bufs.append(b)
        return b

    def ps(self, name, shape, dtype=F32):
        self.nbuf += 1
        shape = list(shape)
        per = 1
        for d in shape[2:]:
            per *= d
        isz = 4 if dtype == F32 else 2
        shape[1] = 2048 // (isz * per)
        b = Buf(self.stack.enter_context(self.nc.psum_tensor(f"{name}_{self.nbuf}", shape, dtype)), name)
        b.psum = True
        return b

    def barrier(self):
        deps = []
        for e in ENGS:
            for o in reversed(self.ops[e]):
                if not o.is_dma:
                    deps.append(o)
                    break
        for b in self.dma_bufs:
            p = Op(None, None)
            p.is_dma = True
            p.dsem = b
            p.dval = b.dma_total
            deps.append(p)
        for e in ENGS:
            self.pending[e] = list(deps)

    @contextlib.contextmanager
    def stage(self):
        prev_stack, prev_bufs = self.stack, self.stage_bufs
        st = contextlib.ExitStack()
        self.stack, self.stage_bufs = st, []
        try:
            yield
        finally:
            self.barrier()
            for b in self.stage_bufs:
                if b.sem is not None:
                    self.sem_pool.append((b.sem, b.dma_total))
            self.stack, self.stage_bufs = prev_stack, prev_bufs
            st.close()

    def dram(self, name, shape, dtype=F32, kind="Internal"):
        return Buf(self.nc.dram_tensor(name, list(shape), dtype, kind=kind), name)

    def _deps(self, op, reads, writes, acc=False):
        deps = op.deps
        pr = [r for r in reads if getattr(r, "psum", False)]
        if pr:
            reads = [r for r in reads if not getattr(r, "psum", False)]
            writes = list(writes) + [r for r in pr if r not in writes]
        for r in reads:
            if r.last_w is not None:
                deps.append(r.last_w)
        for w in writes:
            lw = w.last_w
            if lw is not None and not (acc and lw.eng == op.eng and not lw.is_dma):
                deps.append(lw)
            for rd in w.readers:
                if rd.is_dma or rd.eng != op.eng:
                    deps.append(rd)
        for r in reads:
            r.readers.append(op)
        for w in writes:
            w.last_w = op
            w.readers = []

    def op(self, eng, fn, reads=(), writes=(), acc=False):
        o = Op(eng, fn)
        o.deps.extend(self.pending.pop(eng, ()))
        self._deps(o, reads, writes, acc)
        self.ops[eng].append(o)
        return o

    def dma(self, eng, out_ap, in_ap, semb, reads=(), writes=(), **kw):
        o = Op(eng, lambda e: e.dma_start(out=out_ap, in_=in_ap, **kw))
        o.is_dma = True
        if semb.sem is None:
            if self.sem_pool:
                semb.sem, semb.dma_total = self.sem_pool.pop()
            else:
                semb.sem = self.sem(f"d_{semb.name}_{len(self.dma_bufs)}")
            self.dma_bufs.append(semb)
        semb.dma_total += 16
        o.dsem = semb
        o.dval = semb.dma_total
        o.deps.extend(self.pending.pop(eng, ()))
        self._deps(o, reads, writes)
        self.ops[eng].append(o)
        return o

    def emit(self):
        nc = self.nc
        esem = {e: self.sem(f"e_{e}") for e in ENGS}
        for e in ENGS:
            for o in self.ops[e]:
                for d in o.deps:
                    if not d.is_dma:
                        d.signal = True
        for e in ENGS:
            c = 0
            for o in self.ops[e]:
                if o.signal and not o.is_dma:
                    c += 1
                    o.val = c
        ops = self.ops
        dma_bufs = self.dma_bufs

        def run(e, eng):
            known = {}
            for o in ops[e]:
                need = {}
                for d in o.deps:
                    if d.is_dma:
                        s, v = d.dsem.sem, d.dval
                    else:
                        s, v = esem[d.eng], d.val
                    k = id(s)
                    if k not in need or need[k][1] < v:
                        need[k] = (s, v)
                for k, (s, v) in need.items():
                    if known.get(k, 0) < v:
                        eng.wait_ge(s, v)
                        known[k] = v
                ins = o.fn(eng)
                if o.is_dma:
                    ins.then_inc(o.dsem.sem, 16)
                elif o.signal:
                    ins.then_inc(esem[e], 1)
            if e == "sp":
                for b in dma_bufs:
                    if known.get(id(b.sem), 0) < b.dma_total:
                        eng.wait_ge(b.sem, b.dma_total)

        with nc.Block() as block:
            @block.tensor
            def _(eng):
                run("pe", eng)

            @block.scalar
            def _(eng):
                run("act", eng)

            @block.vector
            def _(eng):
                run("dve", eng)

            @block.gpsimd
            def _(eng):
                run("pool", eng)

            @block.sync
            def _(eng):
                run("sp", eng)
        self.es.close()


def new_nc():
    return bass.Bass("TRN2", target_bir_lowering=False)


def bc_ap(dram_ap_1d, n, parts=128):
    return dram_ap_1d.partition_broadcast(parts)


ADA_COLS = 6 * D // NCORES


def build_ada():
    nc = new_nc()
    k = KB(nc)
    cc = k.dram("cc", [128, KC * 2], F32, kind="ExternalInput")
    w = k.dram("w", [DEPTH * D, ADA_COLS], F32, kind="ExternalInput")
    b = k.dram("b", [DEPTH, ADA_COLS], F32, kind="ExternalInput")
    m = k.dram("m", [DEPTH * 2, ADA_COLS], F32, kind="ExternalOutput")
    cs = k.sb("cs", [128, KC * 2])
    sc = k.sb("sc", [128, KC * 2])
    bs = k.sb("bs", [2, DEPTH * ADA_COLS])
    wb = [k.sb(f"wb{i}", [128, KC, 512]) for i in range(3)]
    ob = [k.sb(f"ob{i}", [2, 512]) for i in range(2)]
    ps = [k.ps(f"ps{i}", [128, 512]) for i in range(2)]
    k.dma("sp", cs[:, :], cc[:, :], cs, writes=[cs])
    for l in range(DEPTH):
        k.dma("sp", bs[:, l * ADA_COLS:(l + 1) * ADA_COLS], b[l, :].partition_broadcast(2), bs, writes=[bs.r(l)])
    k.op("act", lambda e: e.activation(out=sc[:, :], in_=cs[:, :], func=AF.Silu), reads=[cs], writes=[sc])
    it = 0
    for l in range(DEPTH):
        wl = w[l * D:(l + 1) * D, :].rearrange("(j p) n -> p j n", p=128)
        for nb in range(ADA_COLS // 512):
            wt = wb[it % 3]
            pt = ps[it % 2]
            ot = ob[it % 2]
            k.dma("sp" if it % 2 == 0 else "pool", wt[:, :, :], wl[:, :, nb * 512:(nb + 1) * 512], wt, writes=[wt])
            for j in range(KC):
                k.op("pe", lambda e, j=j, wt=wt, pt=pt: e.matmul(pt[0:2, :], sc[:, 2 * j:2 * j + 2], wt[:, j, :],
                                                             start=(j == 0), stop=(j == KC - 1)),
                     reads=[sc, wt], writes=[pt], acc=(j > 0))
            k.op("dve", lambda e, pt=pt, ot=ot, l=l, nb=nb: e.tensor_tensor(
                ot[:, :], pt[0:2, :], bs[:, l * ADA_COLS + nb * 512:l * ADA_COLS + (nb + 1) * 512], ALU.add),
                reads=[pt, bs.r(l)], writes=[ot])
            k.dma("sp", m[2 * l:2 * l + 2, nb * 512:(nb + 1) * 512], ot[:, :], ot, reads=[ot])
            it += 1
    k.emit()
    return nc


def run_ada(inp):
    cc = np.stack([inp["c"][0], inp["c_ctx"]], 0).reshape(2, KC, 128).transpose(2, 1, 0).reshape(128, KC * 2)
    cc = np.ascontiguousarray(cc)
    nc = build_ada()
    maps = []
    for c in range(NCORES):
        sl = slice(c * ADA_COLS, (c + 1) * ADA_COLS)
        maps.append({"cc": cc,
                     "w": np.ascontiguousarray(inp["w_ada"][:, :, sl]).reshape(DEPTH * D, ADA_COLS),
                     "b": np.ascontiguousarray(inp["b_ada"][:, sl])})
    res = run_bass_kernel_spmd(nc, maps, core_ids=list(range(NCORES)))
    m = np.concatenate([r["m"].reshape(DEPTH, 2, ADA_COLS) for r in res.results], axis=2)
    return m


def _copy(e, out, in_):
    if hasattr(e, "tensor_copy"):
        return e.tensor_copy(out, in_)
    return e.copy(out, in_)


class RR:
    def __init__(self, items):
        self.items = list(items)
        self.i = 0

    def __call__(self):
        v = self.items[self.i % len(self.items)]
        self.i += 1
        return v


def load_bc(k, eng, dst, src_1d):
    k.dma(eng, dst[:, :], src_1d.partition_broadcast(dst.t.shape[0]), dst, writes=[dst])


def load_w_bf16(k, wt, src, nk, ncols, stg, cast_eng, dma_eng, res=None, c0=0):
    res = res if res is not None else wt
    step = stg.items[0].t.shape[1]
    for kg in range(0, nk, step):
        kk = min(step, nk - kg)
        st = stg()
        k.dma(dma_eng(), st[:, 0:kk, 0:ncols],
              src[kg * 128:(kg + kk) * 128, :].rearrange("(j p) n -> p j n", p=128), st, writes=[st])
        ce = cast_eng()
        k.op(ce, lambda e, st=st, kg=kg, kk=kk: _copy(e, wt[:, kg:kg + kk, c0:c0 + ncols], st[:, 0:kk, 0:ncols]),
             reads=[st], writes=[res])


def rstd_from_ss(k, out_ap, in_ap, n, bufs_r, bufs_w):
    k.op("dve", lambda e: e.tensor_scalar(out_ap, in_ap, 1.0 / n, EPS, ALU.mult, ALU.add), reads=bufs_r, writes=bufs_w)
    k.op("act", lambda e: e.activation(out=out_ap, in_=out_ap, func=AF.Sqrt), reads=bufs_w, writes=bufs_w)
    k.op("dve", lambda e: e.reciprocal(out_ap, out_ap), reads=bufs_w, writes=bufs_w)


def norm_mod_T(k, xt, G1, sh1, ss, col, tmp, hb, hT_dst, tps, ident, evac):
    k.op("act", lambda e: e.activation(out=tmp[:, :], in_=xt[:, :], func=AF.Square, accum_out=ss[:, col:col + 1]),
         reads=[xt, ss.r("z")], writes=[tmp, ss.r(col)])
    rs = ss[:, col:col + 1]
    rstd_from_ss(k, rs, rs, D, [ss.r(col)], [ss.r(col)])
    k.op("dve", lambda e: e.scalar_tensor_tensor(out=tmp[:, :], in0=xt[:, :], scalar=rs, in1=G1[:, :],
                                                op0=ALU.mult, op1=ALU.mult), reads=[xt, ss.r(col), G1], writes=[tmp])
    k.op("pool", lambda e: e.tensor_tensor(tmp[:, :], tmp[:, :], sh1[:, :], ALU.add), reads=[tmp, sh1], writes=[tmp])
    k.op("act", lambda e: e.copy(hb[:, :], tmp[:, :]), reads=[tmp], writes=[hb])
    for j0 in range(0, KC, 4):
        tp = tps()
        for jj in range(4):
            j = j0 + jj
            k.op("pe", lambda e, tp=tp, jj=jj, j=j: e.transpose(tp[:, jj, :], hb[:, j * 128:(j + 1) * 128], ident[:, :]),
                 reads=[hb, ident], writes=[tp])
        dst, dres = hT_dst(j0, 4)
        k.op(evac(), lambda e, tp=tp, dst=dst: _copy(e, dst, tp[:, 0:4, :]), reads=[tp], writes=dres)


def qk_norm_rope(k, src_ap, nh, gain_bc, csb, kss, kcol, kn, t1, t2, out_bf, res_r, rope=True):
    for h in range(nh):
        k.op("act", lambda e, h=h: e.activation(out=t1[:, 0:128], in_=src_ap[:, h * 128:(h + 1) * 128], func=AF.Square,
                                                 accum_out=kss[:, kcol + h:kcol + h + 1]),
             reads=res_r + [kss.r("z")], writes=[t1, kss.r((kcol, h))])
    rsv = kss[:, kcol:kcol + nh]
    rr = [kss.r((kcol, h)) for h in range(nh)]
    rstd_from_ss(k, rsv, rsv, 128, rr, rr)
    dst = kn if rope else out_bf
    for h in range(nh):
        k.op("dve", lambda e, h=h: e.scalar_tensor_tensor(out=dst[:, h * 128:(h + 1) * 128], in0=src_ap[:, h * 128:(h + 1) * 128],
                                                         scalar=kss[:, kcol + h:kcol + h + 1], in1=gain_bc[:, :],
                                                         op0=ALU.mult, op1=ALU.mult),
             reads=res_r + [kss.r((kcol, h)), gain_bc], writes=[dst])
    if not rope:
        return [out_bf]
    n = nh * 128
    v5 = kn[:, 0:n].rearrange("p (h x y d) -> p h x y d", h=nh, x=2, y=2)
    A, B = v5[:, :, :, 0, :], v5[:, :, :, 1, :]
    o5 = out_bf[:, 0:n].rearrange("p (h x y d) -> p h x y d", h=nh, x=2, y=2)
    oA, oB = o5[:, :, :, 0, :], o5[:, :, :, 1, :]
    C = csb[:, 0:64].rearrange("p (x d) -> p x d", x=2).unsqueeze(1).broadcast_to([128, nh, 2, 32])
    S = csb[:, 64:128].rearrange("p (x d) -> p x d", x=2).unsqueeze(1).broadcast_to([128, nh, 2, 32])
    a4 = t1[:, 0:n // 2].rearrange("p (h x d) -> p h x d", h=nh, x=2)
    b4 = t2[:, 0:n // 2].rearrange("p (h x d) -> p h x d", h=nh, x=2)
    k.op("dve", lambda e: e.tensor_tensor(a4, A, C, ALU.mult), reads=[kn, csb], writes=[t1])
    k.op("pool", lambda e: e.tensor_tensor(b4, B, S, ALU.mult), reads=[kn, csb], writes=[t2])
    k.op("dve", lambda e: e.tensor_tensor(oA, a4, b4, ALU.subtract), reads=[t1, t2], writes=[out_bf.r("A")])
    k.op("dve", lambda e: e.tensor_tensor(a4, B, C, ALU.mult), reads=[kn, csb], writes=[t1])
    k.op("pool", lambda e: e.tensor_tensor(b4, A, S, ALU.mult), reads=[kn, csb], writes=[t2])
    k.op("dve", lambda e: e.tensor_tensor(oB, a4, b4, ALU.add), reads=[t1, t2], writes=[out_bf.r("B")])
    return [out_bf.r("A"), out_bf.r("B")]


NT_LAT = TPC // 128


def build_A():
    nc = new_nc()
    k = KB(nc)
    x = k.dram("x", [TPC, D], F32, kind="ExternalInput")
    vec = k.dram("vec", [3, D], F32, kind="ExternalInput")
    wkv = k.dram("wkv", [D, 1024], F32, kind="ExternalInput")
    wab = k.dram("wab", [D, 1024], F32, kind="ExternalInput")
    kg = k.dram("kg", [2, 128], F32, kind="ExternalInput")
    rope = k.dram("rope", [TPC, 128], F32, kind="ExternalInput")
    idn = k.dram("ident", [128, 128], BF16, kind="ExternalInput")
    kT_o = k.dram("kT", [128, 2, 2, TPC], BF16, kind="ExternalOutput")
    v_o = k.dram("v", [TPC, 2, 256], BF16, kind="ExternalOutput")
    u_o = k.dram("u", [2, 128, GW], F32, kind="ExternalOutput")

    ident = k.sb("ident", [128, 128], BF16)
    gb = k.sb("gb", [128, D])
    G1 = k.sb("G1", [128, D])
    sh1 = k.sb("sh1", [128, D])
    kgs = [k.sb(f"kg{i}", [128, 128]) for i in range(2)]
    ss = k.sb("ss", [128, 64])
    kss = k.sb("kss", [128, 64])
    xts = RR([k.sb(f"xt{i}", [128, D]) for i in range(2)])
    tmp = k.sb("tmp", [128, D])
    hbs = RR([k.sb(f"hb{i}", [128, D], BF16) for i in range(2)])
    hTs = RR([k.sb(f"hT{i}", [128, KC, 128], BF16) for i in range(2)])
    wts = [k.sb(f"wt{i}", [128, KC, 512], BF16) for i in range(2)]
    wa = k.sb("wa", [128, KC, 1024], BF16)
    stg = RR([k.sb(f"stg{i}", [128, 4, 512]) for i in range(2)])
    KT = k.sb("KT", [128, 2, 2, TPC], BF16)
    VS = k.sb("VS", [128, NT_LAT, 2, 256], BF16)
    css = RR([k.sb(f"cs{i}", [128, 128]) for i in range(2)])
    kn = k.sb("kn", [128, 256])
    t1 = k.sb("t1", [128, 256])
    t2 = k.sb("t2", [128, 256])
    kr = RR([k.sb(f"kr{i}", [128, 256], BF16) for i in range(2)])
    sg = k.sb("sg", [128, GW])
    us = RR([k.sb(f"us{i}", [128, GW]) for i in range(2)])
    tps = RR([k.ps(f"tp{i}", [128, 4, 128], BF16) for i in range(2)])
    pks = RR([k.ps(f"pk{i}", [128, 512]) for i in range(2)])
    pab = [k.ps(f"pab{i}", [128, 512]) for i in range(2)]
    evac = RR(["act", "dve"])
    cast = RR(["pool", "dve"])
    dq = RR(["sp", "pool"])

    k.dma("sp", ident[:, :], idn[:, :], ident, writes=[ident])
    load_bc(k, "sp", gb, vec[0, :])
    load_bc(k, "sp", G1, vec[1, :])
    load_bc(k, "sp", sh1, vec[2, :])
    for i in range(2):
        load_bc(k, "sp", kgs[i], kg[i, :])
    k.op("dve", lambda e: e.memset(ss[:, :], 0.0), writes=[ss.r("z")])
    k.op("dve", lambda e: e.memset(kss[:, :], 0.0), writes=[kss.r("z")])
    k.op("dve", lambda e: e.scalar_tensor_tensor(out=G1[:, :], in0=G1[:, :], scalar=1.0, in1=gb[:, :],
                                                op0=ALU.add, op1=ALU.mult), reads=[G1, gb], writes=[G1])
    for i in range(2):
        load_w_bf16(k, wts[i], wkv[:, i * 512:(i + 1) * 512], KC, 512, stg, cast, dq)
    for i in range(2):
        load_w_bf16(k, wa, wab[:, i * 512:(i + 1) * 512], KC, 512, stg, cast, dq, c0=i * 512)

    for t in range(NT_LAT):
        xt = xts()
        hb = hbs()
        hT = hTs()
        k.dma("sp", xt[:, :], x[t * 128:(t + 1) * 128, :], xt, writes=[xt])
        cs = css()
        k.dma("sp", cs[:, :], rope[t * 128:(t + 1) * 128, :], cs, writes=[cs])
        norm_mod_T(k, xt, G1, sh1, ss, t, tmp, hb, lambda j0, n, hT=hT: (hT[:, j0:j0 + n, :], [hT]), tps, ident, evac)
        for g in range(2):
            pk = pks()
            for j in range(KC):
                k.op("pe", lambda e, pk=pk, j=j, g=g, hT=hT: e.matmul(pk[:, :], hT[:, j, :], wts[g][:, j, :],
                                                                   start=(j == 0), stop=(j == KC - 1)),
                     reads=[hT, wts[g]], writes=[pk], acc=(j > 0))
            krb = kr()
            qk_norm_rope(k, pk[:, 0:256], 2, kgs[g], cs, kss, (t * 2 + g) * 2, kn, t1, t2, krb, [pk])
            tp = tps()
            for h in range(2):
                k.op("pe", lambda e, tp=tp, h=h, krb=krb: e.transpose(tp[:, h, :], krb[:, h * 128:(h + 1) * 128], ident[:, :]),
                     reads=[krb.r("A"), krb.r("B"), ident], writes=[tp])
            k.op(evac(), lambda e, tp=tp, g=g, t=t: _copy(e, KT[:, g, :, t * 128:(t + 1) * 128], tp[:, 0:2, :]),
                 reads=[tp], writes=[KT.r(t)])
            k.op(evac(), lambda e, pk=pk, g=g, t=t: _copy(e, VS[:, t, g, :], pk[:, 256:512]),
                 reads=[pk], writes=[VS.r(t)])
        if t in (0, NT_LAT - 1):
            for half in range(2):
                for j in range(KC):
                    k.op("pe", lambda e, j=j, half=half, hT=hT: e.matmul(pab[half][:, :], hT[:, j, :],
                                                                      wa[:, j, half * 512:(half + 1) * 512],
                                                                      start=(j == 0), stop=(j == KC - 1)),
                         reads=[hT, wa], writes=[pab[half]], acc=(j > 0))
            u = us()
            k.op("act", lambda e: e.activation(out=sg[:, :], in_=pab[1][:, :], func=AF.Sigmoid), reads=[pab[1]], writes=[sg])
            k.op("dve", lambda e, u=u: e.tensor_tensor(u[:, :], pab[0][:, :], sg[:, :], ALU.mult), reads=[pab[0], sg], writes=[u])
            k.dma("sp", u_o[0 if t == 0 else 1, :, :], u[:, :], u, reads=[u])
    allKT = [KT.r(t) for t in range(NT_LAT)]
    for g in range(2):
        k.dma(dq(), kT_o[:, g, :, :], KT[:, g, :, :], KT, reads=allKT)
    k.dma("sp", v_o[:, :, :].rearrange("(t p) g c -> p t g c", p=128), VS[:, :, :, :], VS,
          reads=[VS.r(t) for t in range(NT_LAT)])
    k.emit()
    return nc


def rope_table():
    pos = np.arange(SEQ)
    row = (pos // 64).astype(np.float32)
    col = (pos % 64).astype(np.float32)
    inv = (np.float32(10000.0) ** (-np.arange(0, 64, 2, dtype=np.float32) / np.float32(64))).astype(np.float32)
    ar = (row[:, None] * inv[None, :]).astype(np.float32)
    ac = (col[:, None] * inv[None, :]).astype(np.float32)
    return np.concatenate([np.cos(ar), np.cos(ac), np.sin(ar), np.sin(ac)], 1).astype(np.float32)


NTOK = NCTX + TPC
NTILE = NTOK // 128
V_G1, V_SC1, V_SH1, V_GT1, V_SC2, V_SH2, V_GT2 = 0, 1, 2, 3, 4, 5, 6
V_CTX = 6
V_GBR, V_G2 = 13, 14
GELU_C = 2.0 * math.sqrt(2.0 / math.pi)


def mm_acc(k, out_ap, out_res, pairs, reads):
    n = len(pairs)
    for i, (l, r) in enumerate(pairs):
        k.op("pe", lambda e, l=l, r=r, i=i: e.matmul(out_ap, l, r, start=(i == 0), stop=(i == n - 1)),
             reads=reads, writes=[out_res], acc=(i > 0))


def stage_proj(k, c):
    with k.stage():
        ident = k.sb("ident", [128, 128], BF16)
        k.dma("sp", ident[:, :], c["ident"][:, :], ident, writes=[ident])
        gb = k.sb("gb", [128, D])
        G1 = k.sb("G1", [128, D])
        sh1 = k.sb("sh1", [128, D])
        ss = k.sb("ss", [128, 32])
        xts = RR([k.sb(f"xt{i}", [128, D]) for i in range(2)])
        tmp = k.sb("tmp", [128, D])
        hbs = RR([k.sb(f"hb{i}", [128, D], BF16) for i in range(2)])
        hT = k.sb("hTall", [128, KC, NTOK], BF16)
        wts = RR([k.sb(f"wt{i}", [128, KC, 512], BF16) for i in range(2)])
        stg = RR([k.sb(f"stg{i}", [128, 4, 512]) for i in range(2)])
        pos = RR([k.sb(f"po{i}", [128, 512]) for i in range(3)])
        tps = RR([k.ps(f"tp{i}", [128, 4, 128], BF16) for i in range(2)])
        pps = RR([k.ps(f"pp{i}", [128, 512]) for i in range(4)])
        evac = RR(["act", "dve"])
        cast = RR(["pool", "dve"])
        dq = RR(["sp", "pool"])
        vec = c["vec"]
        k.op("dve", lambda e: e.memset(ss[:, :], 0.0), writes=[ss.r("z")])
        load_bc(k, "sp", gb, vec[V_G1, :])
        for t in range(NTILE):
            if t in (0, NCTX // 128):
                off = V_CTX if t == 0 else 0
                load_bc(k, "sp", G1, vec[V_SC1 + off, :])
                load_bc(k, "sp", sh1, vec[V_SH1 + off, :])
                k.op("dve", lambda e: e.scalar_tensor_tensor(out=G1[:, :], in0=G1[:, :], scalar=1.0, in1=gb[:, :],
                                                            op0=ALU.add, op1=ALU.mult), reads=[G1, gb], writes=[G1])
            xt = xts()
            k.dma(dq(), xt[:, :], c["x"][t * 128:(t + 1) * 128, :], xt, writes=[xt])
            norm_mod_T(k, xt, G1, sh1, ss, t, tmp, hbs(),
                       lambda j0, n, t=t: (hT[:, j0:j0 + n, t * 128:(t + 1) * 128], [hT.r(t)]), tps, ident, evac)
        for cb in range(IN_W // 512):
            wt = wts()
            load_w_bf16(k, wt, c["w_in"][:, cb * 512:(cb + 1) * 512], KC, 512, stg, cast, dq)
            for t in range(NTILE):
                pp = pps()
                mm_acc(k, pp[:, :], pp, [(hT[:, j, t * 128:(t + 1) * 128], wt[:, j, :]) for j in range(KC)], [hT.r(t), wt])
                po = pos()
                k.op(evac(), lambda e, po=po, pp=pp: _copy(e, po[:, :], pp[:, :]), reads=[pp], writes=[po])
                k.dma(dq(), c["P"][t * 128:(t + 1) * 128, cb * 512:(cb + 1) * 512], po[:, :], po, reads=[po])


def stage_conv(k, c):
    with k.stage():
        idf = k.sb("idf", [128, 128])
        k.dma("sp", idf[:, :], c["identf"][:, :], idf, writes=[idf])
        ones = k.sb("ones", [128, 128])
        k.op("dve", lambda e: e.memset(ones[:, :], 1.0), writes=[ones])
        cp = k.sb("cpar", [128, 136])
        k.dma("sp", cp[:, :], c["cpar"][:, :], cp, writes=[cp])
        pwb = k.sb("pwb", [128, GW])
        load_bc(k, "sp", pwb, c["pvec"][0, :])
        pw = k.sb("pw", [128, 4, GW], BF16)
        stg = RR([k.sb(f"stg{i}", [128, 4, 512]) for i in range(1)])
        load_w_bf16(k, pw, c["conv_pw"][:, :], 4, GW, stg, RR(["dve"]), RR(["sp"]))
        NL = TPC
        UT = k.sb("UT", [128, 4, NL + 2 * HALO])
        acc = k.sb("acc", [128, 4, NL])
        ctmp = k.sb("ctmp", [128, NL])
        abs_ = RR([k.sb(f"ab{i}", [128, 1024]) for i in range(2)])
        sg = k.sb("sg", [128, GW])
        us = RR([k.sb(f"u{i}", [128, GW]) for i in range(2)])
        sq = k.sb("sq", [128, 4, 512])
        mean = k.sb("mean", [128, 512])
        rstd = k.sb("rstd", [128, 512])
        zt = k.sb("zt", [128, 512])
        sT = k.sb("sT", [128, 4, 512], BF16)
        ycs = RR([k.sb(f"yc{i}", [128, GW]) for i in range(2)])
        tpf = RR([k.ps(f"tpf{i}", [128, 4, 128]) for i in range(2)])
        pst = [k.ps(f"pst{i}", [128, 512]) for i in range(2)]
        pys = RR([k.ps(f"py{i}", [128, 512]) for i in range(2)])
        dq = RR(["sp", "pool"])
        for (t0, nt, halo) in ((0, NCTX // 128, False), (NCTX // 128, NT_LAT, True)):
            N = nt * 128
            if halo:
                for side in range(2):
                    hp = us()
                    k.op("dve", lambda e, hp=hp: e.memset(hp[:, :], 0.0), writes=[hp])
                    k.dma("sp", hp[0:HALO, :], c["uh"][side, :, :], hp, writes=[hp])
                    tp = tpf()
                    for cc in range(4):
                        k.op("pe", lambda e, tp=tp, cc=cc, hp=hp: e.transpose(tp[:, cc, :], hp[:, cc * 128:(cc + 1) * 128], idf[:, :]),
                             reads=[hp, idf], writes=[tp])
                    c0 = 0 if side == 0 else HALO + N
                    k.op("act", lambda e, tp=tp, c0=c0: e.copy(UT[:, :, c0:c0 + HALO], tp[:, :, 0:HALO]),
                         reads=[tp], writes=[UT.r(("h", side))])
            else:
                k.op("dve", lambda e: e.memset(UT[:, :, 0:HALO], 0.0), writes=[UT.r(("h", 0))])
                k.op("dve", lambda e, N=N: e.memset(UT[:, :, HALO + N:2 * HALO + N], 0.0), writes=[UT.r(("h", 1))])
            for ti in range(nt):
                t = t0 + ti
                ab = abs_()
                k.dma(dq(), ab[:, :], c["P"][t * 128:(t + 1) * 128, 0:1024], ab, writes=[ab])
                k.op("act", lambda e, ab=ab: e.activation(out=sg[:, :], in_=ab[:, 512:1024], func=AF.Sigmoid), reads=[ab], writes=[sg])
                u = us()
                k.op("dve", lambda e, ab=ab, u=u: e.tensor_tensor(u[:, :], ab[:, 0:512], sg[:, :], ALU.mult), reads=[ab, sg], writes=[u])
                tp = tpf()
                for cc in range(4):
                    k.op("pe", lambda e, tp=tp, cc=cc, u=u: e.transpose(tp[:, cc, :], u[:, cc * 128:(cc + 1) * 128], idf[:, :]),
                         reads=[u, idf], writes=[tp])
                k.op("act", lambda e, tp=tp, ti=ti: e.copy(UT[:, :, HALO + ti * 128:HALO + (ti + 1) * 128], tp[:, 0:4, :]),
                     reads=[tp], writes=[UT.r(ti)])
            allU = [UT.r(ti) for ti in range(nt)] + [UT.r(("h", 0)), UT.r(("h", 1))]
            for tap in range(CONV_W):
                for cc in range(4):
                    eng = "dve" if cc < 3 else "pool"
                    w_ap = cp[:, cc * 31 + tap:cc * 31 + tap + 1]
                    if tap == 0:
                        k.op(eng, lambda e, cc=cc, w_ap=w_ap, N=N: e.tensor_scalar(
                            acc[:, cc, 0:N], UT[:, cc, 0:N], w_ap, cp[:, 124 + cc:125 + cc], ALU.mult, ALU.add),
                            reads=allU + [cp], writes=[acc.r(cc)])
                    elif eng == "dve":
                        k.op(eng, lambda e, cc=cc, w_ap=w_ap, tap=tap, N=N: e.scalar_tensor_tensor(
                            out=acc[:, cc, 0:N], in0=UT[:, cc, tap:tap + N], scalar=w_ap, in1=acc[:, cc, 0:N],
                            op0=ALU.mult, op1=ALU.add), reads=allU + [cp, acc.r(cc)], writes=[acc.r(cc)])
                    else:
                        k.op(eng, lambda e, cc=cc, w_ap=w_ap, tap=tap, N=N: e.tensor_scalar(
                            ctmp[:, 0:N], UT[:, cc, tap:tap + N], w_ap, 0.0, ALU.mult, ALU.add), reads=allU + [cp], writes=[ctmp])
                        k.op(eng, lambda e, cc=cc, N=N: e.tensor_tensor(acc[:, cc, 0:N], acc[:, cc, 0:N], ctmp[:, 0:N], ALU.add),
                             reads=[ctmp, acc.r(cc)], writes=[acc.r(cc)])
            accr = [acc.r(cc) for cc in range(4)]
            for b0 in range(0, N, 512):
                nb = min(512, N - b0)
                k.op("act", lambda e, b0=b0, nb=nb: e.activation(out=sq[:, :, 0:nb], in_=acc[:, :, b0:b0 + nb], func=AF.Square),
                     reads=accr, writes=[sq])
                mm_acc(k, pst[0][:, 0:nb], pst[0], [(ones[:, :], acc[:, cc, b0:b0 + nb]) for cc in range(4)], accr + [ones])
                mm_acc(k, pst[1][:, 0:nb], pst[1], [(ones[:, :], sq[:, cc, 0:nb]) for cc in range(4)], [sq, ones])
                k.op("act", lambda e, nb=nb: e.activation(out=mean[:, 0:nb], in_=pst[0][:, 0:nb], func=AF.Copy, scale=1.0 / GW),
                     reads=[pst[0]], writes=[mean])
                k.op("dve", lambda e, nb=nb: e.tensor_tensor(zt[:, 0:nb], mean[:, 0:nb], mean[:, 0:nb], ALU.mult), reads=[mean], writes=[zt])
                k.op("dve", lambda e, nb=nb: e.scalar_tensor_tensor(out=rstd[:, 0:nb], in0=pst[1][:, 0:nb], scalar=1.0 / GW, in1=zt[:, 0:nb],
                                                                  op0=ALU.mult, op1=ALU.subtract), reads=[pst[1], zt], writes=[rstd])
                k.op("dve", lambda e, nb=nb: e.tensor_scalar(rstd[:, 0:nb], rstd[:, 0:nb], 1.0, EPS, ALU.mult, ALU.add), reads=[rstd], writes=[rstd])
                k.op("act", lambda e, nb=nb: e.activation(out=rstd[:, 0:nb], in_=rstd[:, 0:nb], func=AF.Sqrt), reads=[rstd], writes=[rstd])
                k.op("dve", lambda e, nb=nb: e.reciprocal(rstd[:, 0:nb], rstd[:, 0:nb]), reads=[rstd], writes=[rstd])
                for cc in range(4):
                    k.op("dve", lambda e, cc=cc, b0=b0, nb=nb: e.tensor_tensor(zt[:, 0:nb], acc[:, cc, b0:b0 + nb], mean[:, 0:nb], ALU.subtract),
                         reads=[acc.r(cc), mean], writes=[zt])
                    k.op("pool", lambda e, nb=nb: e.tensor_tensor(zt[:, 0:nb], zt[:, 0:nb], rstd[:, 0:nb], ALU.mult), reads=[zt, rstd], writes=[zt])
                    k.op("dve", lambda e, cc=cc, nb=nb: e.tensor_scalar(zt[:, 0:nb], zt[:, 0:nb], cp[:, 128 + cc:129 + cc], cp[:, 132 + cc:133 + cc],
                                                                      ALU.mult, ALU.add), reads=[zt, cp], writes=[zt])
                    k.op("act", lambda e, cc=cc, nb=nb: e.activation(out=sT[:, cc, 0:nb], in_=zt[:, 0:nb], func=AF.Silu), reads=[zt], writes=[sT.r(cc)])
                sTr = [sT.r(cc) for cc in range(4)]
                for sub in range(nb // 128):
                    py = pys()
                    mm_acc(k, py[:, :], py, [(sT[:, cc, sub * 128:(sub + 1) * 128], pw[:, cc, :]) for cc in range(4)], sTr + [pw])
                    yc = ycs()
                    k.op("dve", lambda e, py=py, yc=yc: e.tensor_tensor(yc[:, :], py[:, :], pwb[:, :], ALU.add), reads=[py, pwb], writes=[yc])
                    r0 = t0 * 128 + b0 + sub * 128
                    k.dma(dq(), c["Y"][r0:r0 + 128, 0:GW], yc[:, :], yc, reads=[yc])


def stage_sgu(k, c):
    with k.stage():
        lg = k.sb("lg", [128, GW])
        lb = k.sb("lb", [128, GW])
        load_bc(k, "sp", lg, c["pvec"][1, :])
        load_bc(k, "sp", lb, c["pvec"][2, :])
        bsT = k.sb("bsT", [128, 4])
        k.dma("sp", bsT[:, :], c["bsT"][:, :], bsT, writes=[bsT])
        wsf = k.sb("wsf", [128, 4, 128])
        ws = k.sb("ws", [128, 4, 128], BF16)
        k.dma("sp", wsf[:, :, :], c["sgw"][:, :, :].rearrange("g q p -> q g p"), wsf, writes=[wsf])
        k.op("dve", lambda e: e.tensor_copy(ws[:, :, :], wsf[:, :, :]), reads=[wsf], writes=[ws])
        st = k.sb("st", [128, 64])
        k.op("dve", lambda e: e.memset(st[:, :], 0.0), writes=[st.r("z")])
        pzs = RR([k.sb(f"pz{i}", [128, 1024]) for i in range(2)])
        t1 = k.sb("t1", [128, 1024])
        zz = k.sb("zz", [128, 1024])
        vn = k.sb("vn", [128, GW])
        vbs = RR([k.sb(f"vb{i}", [128, GW], BF16) for i in range(2)])
        ys = RR([k.sb(f"ys{i}", [128, GW]) for i in range(2)])
        pss = RR([k.ps(f"ps{i}", [128, 512]) for i in range(2)])
        dq = RR(["sp", "pool"])
        for t in range(NTILE):
            pz = pzs()
            k.dma(dq(), pz[:, :], c["P"][t * 128:(t + 1) * 128, 1024:2048], pz, writes=[pz])
            k.op("pool", lambda e, pz=pz: e.tensor_tensor(t1[:, :], pz[:, :], pz[:, :], ALU.mult), reads=[pz], writes=[t1])
            k.op("dve", lambda e: e.tensor_scalar(t1[:, :], t1[:, :], 0.044715, 1.0, ALU.mult, ALU.add), reads=[t1], writes=[t1])
            k.op("pool", lambda e, pz=pz: e.tensor_tensor(t1[:, :], t1[:, :], pz[:, :], ALU.mult), reads=[t1, pz], writes=[t1])
            k.op("act", lambda e: e.activation(out=t1[:, :], in_=t1[:, :], func=AF.Sigmoid, scale=GELU_C), reads=[t1], writes=[t1])
            k.op("dve", lambda e, pz=pz: e.tensor_tensor(zz[:, :], pz[:, :], t1[:, :], ALU.mult), reads=[pz, t1], writes=[zz])
            c0 = 3 * t
            k.op("dve", lambda e, c0=c0: e.reduce_sum(out=st[:, c0:c0 + 1], in_=zz[:, 512:1024], axis=AX.X), reads=[zz, st.r("z")], writes=[st.r(c0)])
            k.op("act", lambda e, c0=c0: e.activation(out=vn[:, :], in_=zz[:, 512:1024], func=AF.Square, accum_out=st[:, c0 + 1:c0 + 2]),
                 reads=[zz, st.r("z")], writes=[vn, st.r(c0 + 1)])
            mean, ex2, var = st[:, c0:c0 + 1], st[:, c0 + 1:c0 + 2], st[:, c0 + 2:c0 + 3]
            rs = [st.r(c0), st.r(c0 + 1), st.r(c0 + 2)]
            k.op("dve", lambda e, mean=mean: e.tensor_scalar(mean, mean, 1.0 / GW, 0.0, ALU.mult, ALU.add), reads=[st.r(c0)], writes=[st.r(c0)])
            k.op("dve", lambda e, mean=mean, var=var: e.tensor_tensor(var, mean, mean, ALU.mult), reads=[st.r(c0), st.r("z")], writes=[st.r(c0 + 2)])
            k.op("dve", lambda e, ex2=ex2, var=var: e.scalar_tensor_tensor(out=var, in0=ex2, scalar=1.0 / GW, in1=var, op0=ALU.mult, op1=ALU.subtract),
                 reads=rs, writes=[st.r(c0 + 2)])
            k.op("dve", lambda e, var=var: e.tensor_scalar(var, var, 1.0, EPS, ALU.mult, ALU.add), reads=[st.r(c0 + 2)], writes=[st.r(c0 + 2)])
            k.op("act", lambda e, var=var: e.activation(out=var, in_=var, func=AF.Sqrt), reads=[st.r(c0 + 2)], writes=[st.r(c0 + 2)])
            k.op("dve", lambda e, var=var: e.reciprocal(var, var), reads=[st.r(c0 + 2)], writes=[st.r(c0 + 2)])
            k.op("dve", lambda e, mean=mean, var=var: e.tensor_scalar(vn[:, :], zz[:, 512:1024], mean, var, ALU.subtract, ALU.mult),
                 reads=[zz] + rs, writes=[vn])
            k.op("pool", lambda e: e.tensor_tensor(vn[:, :], vn[:, :], lg[:, :], ALU.mult), reads=[vn, lg], writes=[vn])
            vb = vbs()
            k.op("dve", lambda e, vb=vb: e.tensor_tensor(vb[:, :], vn[:, :], lb[:, :], ALU.add), reads=[vn, lb], writes=[vb])
            ps = pss()
            for g in range(4):
                k.op("pe", lambda e, g=g, ps=ps, vb=vb: e.matmul(ps[:, g * 128:(g + 1) * 128], ws[:, g, :], vb[:, g * 128:(g + 1) * 128],
                                                               start=True, stop=True), reads=[ws, vb], writes=[ps.r(g)])
            y = ys()
            for g in range(4):
                k.op("dve", lambda e, g=g, ps=ps, y=y: e.scalar_tensor_tensor(
                    out=y[:, g * 128:(g + 1) * 128], in0=ps[:, g * 128:(g + 1) * 128], scalar=bsT[:, g:g + 1],
                    in1=zz[:, g * 128:(g + 1) * 128], op0=ALU.add, op1=ALU.mult), reads=[ps.r(g), bsT, zz], writes=[y.r(g)])
            k.dma(dq(), c["Y"][t * 128:(t + 1) * 128, GW:2 * GW], y[:, :], y, reads=[y.r(g) for g in range(4)])


NCHUNK_G = 8


def stage_attn(k, c):
    with k.stage():
        ident = k.sb("ident", [128, 128], BF16)
        k.dma("sp", ident[:, :], c["ident"][:, :], ident, writes=[ident])
        gn = [k.sb(f"gn{i}", [128, 128]) for i in range(4)]
        for i in range(4):
            load_bc(k, "sp", gn[i], c["gains"][i, :])
        sinkb = k.sb("sinkb", [128, 4])
        load_bc(k, "sp", sinkb, c["sink"][0, :])
        mx = k.sb("mx", [128, 4])
        negb = k.sb("negb", [128, 2])
        es = k.sb("es", [128, 8])
        ga = k.sb("ga", [128, 128])
        for i in range(4):
            k.op("dve", lambda e, i=i: e.tensor_tensor(ga[:, :], gn[i][:, :], gn[i][:, :], ALU.mult), reads=[gn[i]], writes=[ga])
            k.op("dve", lambda e, i=i: e.reduce_max(out=mx[:, i:i + 1], in_=ga[:, :], axis=AX.X), reads=[ga], writes=[mx.r(i)])
        for g in range(2):
            k.op("dve", lambda e, g=g: e.tensor_tensor(negb[:, g:g + 1], mx[:, 2 * g:2 * g + 1], mx[:, 2 * g + 1:2 * g + 2], ALU.mult),
                 reads=[mx.r(2 * g), mx.r(2 * g + 1)], writes=[negb.r(g)])
            k.op("act", lambda e, g=g: e.activation(out=negb[:, g:g + 1], in_=negb[:, g:g + 1], func=AF.Sqrt), reads=[negb.r(g)], writes=[negb.r(g)])
            k.op("dve", lambda e, g=g: e.tensor_scalar(negb[:, g:g + 1], negb[:, g:g + 1], -SCALE * 128.0, 0.0, ALU.mult, ALU.add),
                 reads=[negb.r(g)], writes=[negb.r(g)])
        k.op("dve", lambda e: e.memset(es[:, :], 0.0), writes=[es])
        k.op("act", lambda e: e.activation(out=es[:, 0:4], in_=sinkb[:, :], func=AF.Exp, bias=negb[:, 0:1], scale=1.0),
             reads=[sinkb, negb.r(0), es], writes=[es])
        masks = [k.sb(f"mask{i}", [128, 128], BF16) for i in range(4)]
        for i in range(4):
            k.dma("sp", masks[i][:, :], c["masks"][i, :, :], masks[i], writes=[masks[i]])
        QT = k.sb("QT", [128, 2, 4, NTOK], BF16)
        KTc = k.sb("KTc", [128, 2, 2, NCTX], BF16)
        Vc = k.sb("Vc", [128, 2, 2, 2, 129], BF16)
        k.op("pool", lambda e: e.memset(Vc[:, :, :, :, :], 1.0), writes=[Vc])
        KTw = k.sb("KTw", [128, 2, NTOK], BF16)
        Vw = k.sb("Vw", [128, NTILE, 2, 129], BF16)
        k.dma("sp", KTw[:, :, :], c["KTs"][:, :, :], KTw, writes=[KTw])
        k.dma("pool", Vw[:, :, :, :], c["Vs"][:, :, :].rearrange("(b p) h c -> p b h c", p=128), Vw, writes=[Vw])
        kss = k.sb("kss", [128, 256])
        k.op("dve", lambda e: e.memset(kss[:, :], 0.0), writes=[kss.r("z")])
        pqs = RR([k.sb(f"pq{i}", [128, 2048]) for i in range(2)])
        css = RR([k.sb(f"cs{i}", [128, 128]) for i in range(2)])
        kn = k.sb("kn", [128, 512])
        t1 = k.sb("t1", [128, 256])
        t2 = k.sb("t2", [128, 256])
        qrs = RR([k.sb(f"qr{i}", [128, 512], BF16) for i in range(2)])
        tps = RR([k.ps(f"tp{i}", [128, 4, 128], BF16) for i in range(1)])
        evac = RR(["act", "dve"])
        dq = RR(["sp", "pool"])
        for t in range(NTILE):
            is_ctx = t < NCTX // 128
            pq = pqs()
            k.dma(dq(), pq[:, :], c["P"][t * 128:(t + 1) * 128, 2048:4096], pq, writes=[pq])
            cs = css()
            if not is_ctx:
                tl = t - NCTX // 128
                k.dma("sp", cs[:, :], c["rope"][tl * 128:(tl + 1) * 128, :], cs, writes=[cs])
            for g in range(2):
                base = g * 1024
                qr = qrs()
                rr = qk_norm_rope(k, pq[:, base:base + 512], 4, gn[2 * g], cs, kss, t * 12 + g * 6, kn, t1, t2, qr, [pq], rope=not is_ctx)
                tp = tps()
                for h in range(4):
                    k.op("pe", lambda e, tp=tp, h=h, qr=qr: e.transpose(tp[:, h, :], qr[:, h * 128:(h + 1) * 128], ident[:, :]),
                         reads=rr + [ident], writes=[tp])
                k.op(evac(), lambda e, tp=tp, g=g, t=t: _copy(e, QT[:, g, :, t * 128:(t + 1) * 128], tp[:, 0:4, :]),
                     reads=[tp], writes=[QT.r((g, t))])
                if is_ctx:
                    qr = qrs()
                    rr = qk_norm_rope(k, pq[:, base + 512:base + 768], 2, gn[2 * g + 1], cs, kss, t * 12 + g * 6 + 4, kn, t1, t2, qr, [pq], rope=False)
                    tp = tps()
                    for h in range(2):
                        k.op("pe", lambda e, tp=tp, h=h, qr=qr: e.transpose(tp[:, h, :], qr[:, h * 128:(h + 1) * 128], ident[:, :]),
                             reads=rr + [ident], writes=[tp])
                    k.op(evac(), lambda e, tp=tp, g=g, t=t: _copy(e, KTc[:, g, :, t * 128:(t + 1) * 128], tp[:, 0:2, :]),
                         reads=[tp], writes=[KTc.r((g, t))])
                    k.op("dve", lambda e, pq=pq, g=g, t=t, base=base: e.tensor_copy(
                        Vc[:, t, g, :, 0:128], pq[:, base + 768:base + 1024].rearrange("p (h d) -> p h d", h=2)),
                        reads=[pq, Vc], writes=[Vc.r((g, t))])
        sts = RR([k.ps(f"st{i}", [128, 512]) for i in range(2)])
        oacc = [k.ps(f"oa{i}", [128, 512]) for i in range(4)]
        pts = RR([k.sb(f"pt{i}", [128, 256], BF16) for i in range(3)])
        ys = RR([k.sb(f"ya{i}", [128, GW]) for i in range(2)])
        dn = k.sb("dn", [128, 8])
        KTgs = RR([k.sb(f"KTg{i}", [128, 2, 2048], BF16) for i in range(2)])
        Vgs = RR([k.sb(f"Vg{i}", [128, 16, 2, 129], BF16) for i in range(2)])
        nctx_b = NCTX // 128

        def att_block(g, t, kt_fn, v_fn, mask, first, last, reads_kv):
            for kvh in range(2):
                st = sts()
                k.op("pe", lambda e, st=st, kvh=kvh: e.matmul(st[:, 0:256], kt_fn(kvh), QT[:, g, 2 * kvh:2 * kvh + 2, t * 128:(t + 1) * 128],
                                                            start=True, stop=True), reads=reads_kv + [QT.r((g, t))], writes=[st])
                pt = pts()
                k.op("act", lambda e, st=st, pt=pt: e.activation(out=pt[:, :], in_=st[:, 0:256], func=AF.Exp, bias=negb[:, g:g + 1], scale=SCALE),
                     reads=[st, negb.r(g)], writes=[pt])
                if mask is not None:
                    k.op("dve", lambda e, pt=pt: e.tensor_tensor(pt[:, :].rearrange("p (h q) -> p h q", h=2),
                                                               pt[:, :].rearrange("p (h q) -> p h q", h=2),
                                                               mask[:, :].unsqueeze(1).broadcast_to([128, 2, 128]), ALU.mult),
                         reads=[pt, mask], writes=[pt])
                for hh in range(2):
                    oa = oacc[kvh * 2 + hh]
                    k.op("pe", lambda e, oa=oa, pt=pt, hh=hh, kvh=kvh: e.matmul(oa[:, 0:129], pt[:, hh * 128:(hh + 1) * 128], v_fn(kvh),
                                                                              start=first, stop=last),
                         reads=reads_kv + [pt], writes=[oa], acc=not first)

        def finalize(g, t):
            y = ys()
            for h in range(4):
                oa = oacc[h]
                dcol = dn[:, h:h + 1]
                k.op("dve", lambda e, oa=oa, dcol=dcol, h=h: e.tensor_scalar(dcol, oa[:, 128:129], es[:, g * 4 + h:g * 4 + h + 1], 0.0, ALU.add, ALU.add),
                     reads=[oa, es], writes=[dn.r(h)])
                k.op("dve", lambda e, dcol=dcol: e.reciprocal(dcol, dcol), reads=[dn.r(h)], writes=[dn.r(h)])
                k.op("dve", lambda e, oa=oa, dcol=dcol, h=h, y=y: e.tensor_scalar(y[:, h * 128:(h + 1) * 128], oa[:, 0:128], dcol, 0.0, ALU.mult, ALU.add),
                     reads=[oa, dn.r(h)], writes=[y.r(h)])
            k.dma(dq(), c["Y"][t * 128:(t + 1) * 128, 1024 + g * GW:1024 + (g + 1) * GW], y[:, :], y, reads=[y.r(h) for h in range(4)])

        ctx_res = lambda g: [KTc.r((g, b)) for b in range(nctx_b)] + [Vc.r((g, b)) for b in range(nctx_b)]
        for t in range(NTILE):
            is_ctx = t < nctx_b
            i = t - nctx_b
            blocks = [("c", b, None) for b in range(nctx_b)]
            if not is_ctx:
                blocks += [("w", i, masks[2] if i == 0 else masks[0]), ("w", i + 1, None),
                           ("w", i + 2, masks[3] if i == NT_LAT - 1 else masks[1])]
            for bi, (kind, b, mask) in enumerate(blocks):
                if kind == "c":
                    att_block(0, t, lambda kvh, b=b: KTc[:, 0, kvh, b * 128:(b + 1) * 128], lambda kvh, b=b: Vc[:, b, 0, kvh, :],
                              mask, bi == 0, bi == len(blocks) - 1, ctx_res(0))
                else:
                    att_block(0, t, lambda kvh, b=b: KTw[:, kvh, b * 128:(b + 1) * 128], lambda kvh, b=b: Vw[:, b, kvh, :],
                              mask, bi == 0, bi == len(blocks) - 1, [KTw, Vw])
            finalize(0, t)
            nblk = nctx_b + (0 if is_ctx else SEQ // 128)
            for b in range(nctx_b):
                att_block(1, t, lambda kvh, b=b: KTc[:, 1, kvh, b * 128:(b + 1) * 128], lambda kvh, b=b: Vc[:, b, 1, kvh, :],
                          None, b == 0, b == nblk - 1, ctx_res(1))
            if not is_ctx:
                for ci in range(NCHUNK_G):
                    KTg, Vg = KTgs(), Vgs()
                    k.dma("sp", KTg[:, :, :], c["KTg"][:, :, ci * 2048:(ci + 1) * 2048], KTg, writes=[KTg])
                    k.dma("pool", Vg[:, :, :, :], c["Vg"][ci * 2048:(ci + 1) * 2048, :, :].rearrange("(b p) h c -> p b h c", p=128), Vg, writes=[Vg])
                    for bb in range(16):
                        bidx = nctx_b + ci * 16 + bb
                        att_block(1, t, lambda kvh, bb=bb, KTg=KTg: KTg[:, kvh, bb * 128:(bb + 1) * 128],
                                  lambda kvh, bb=bb, Vg=Vg: Vg[:, bb, kvh, :], None, False, bidx == nblk - 1, [KTg, Vg])
            finalize(1, t)


def router_tile(k, L, plg, brb, c, t):
    lg, m1, m2, mk1, mk2, l2, ed, g1, g2, gate = (L[:, 0:8], L[:, 8:9], L[:, 9:10], L[:, 16:24], L[:, 24:32], L[:, 32:40],
                                                 L[:, 10:11], L[:, 11:12], L[:, 12:13], L[:, 40:48])
    ops = [
        lambda e: e.tensor_tensor(lg, plg[:, 0:NEXP], brb[:, :], ALU.add),
        lambda e: e.reduce_max(out=m1, in_=lg, axis=AX.X),
        lambda e: e.tensor_scalar(mk1, lg, m1, 0.0, ALU.is_equal, ALU.add),
        lambda e: e.scalar_tensor_tensor(out=l2, in0=mk1, scalar=-1e30, in1=lg, op0=ALU.mult, op1=ALU.add),
        lambda e: e.reduce_max(out=m2, in_=l2, axis=AX.X),
        lambda e: e.tensor_scalar(mk2, l2, m2, 0.0, ALU.is_equal, ALU.add),
        lambda e: e.tensor_tensor(ed, m2, m1, ALU.subtract),
    ]
    for f in ops:
        k.op("dve", f, reads=[plg, brb, L], writes=[L])
    k.op("act", lambda e: e.activation(out=ed, in_=ed, func=AF.Exp), reads=[L], writes=[L])
    ops = [
        lambda e: e.tensor_scalar(g1, ed, 1.0, 0.0, ALU.add, ALU.add),
        lambda e: e.reciprocal(g1, g1),
        lambda e: e.tensor_tensor(g2, ed, g1, ALU.mult),
        lambda e: e.tensor_scalar(gate, mk1, g1, 0.0, ALU.mult, ALU.add),
        lambda e: e.scalar_tensor_tensor(out=gate, in0=mk2, scalar=g2, in1=gate, op0=ALU.mult, op1=ALU.add),
    ]
    for f in ops:
        k.op("dve", f, reads=[L], writes=[L])
    k.dma("sp", c["gates"][t * 128:(t + 1) * 128, :], gate, L, reads=[L])


def stage_out(k, c, moe):
    with k.stage():
        ident = k.sb("ident", [128, 128], BF16)
        k.dma("sp", ident[:, :], c["ident"][:, :], ident, writes=[ident])
        vec = c["vec"]
        wo = k.sb("wo", [128, KC, D], BF16)
        stg = RR([k.sb(f"stg{i}", [128, 4, 512]) for i in range(2)])
        cast = RR(["pool", "dve"])
        dq = RR(["sp", "pool"])
        for cb in range(4):
            load_w_bf16(k, wo, c["w_out"][:, cb * 512:(cb + 1) * 512], KC, 512, stg, cast, dq, res=wo.r(cb), c0=cb * 512)
        gbr = k.sb("gbr", [128, D])
        load_bc(k, "sp", gbr, vec[V_GBR, :])
        g2b = k.sb("g2b", [128, D])
        load_bc(k, "sp", g2b, vec[V_G2, :])
        gt1 = k.sb("gt1", [128, D])
        G2 = k.sb("G2", [128, D])
        sh2 = k.sb("sh2", [128, D])
        gs = k.sb("gs", [128, 128])
        ss2 = k.sb("ss2", [128, 32])
        k.op("dve", lambda e: e.memset(gs[:, :], 0.0), writes=[gs.r("z")])
        k.op("dve", lambda e: e.memset(ss2[:, :], 0.0), writes=[ss2.r("z")])
        xt = k.sb("xt", [128, D])
        yt = k.sb("yt", [128, D])
        x1 = k.sb("x1", [128, D])
        tmp = k.sb("tmp", [128, D])
        tmc = k.sb("tmc", [128, 512])
        hb = k.sb("hb", [128, D], BF16)
        yTs = RR([k.sb(f"yT{i}", [128, KC, 128], BF16) for i in range(2)])
        h2s = RR([k.sb(f"h2{i}", [128, KC, 128], BF16) for i in range(2)])
        tps = RR([k.ps(f"tp{i}", [128, 4, 128], BF16) for i in range(2)])
        pps = RR([k.ps(f"pp{i}", [128, 512]) for i in range(3)])
        evac = RR(["act", "dve"])
        if moe:
            idf = k.sb("idf", [128, 128])
            k.dma("sp", idf[:, :], c["identf"][:, :], idf, writes=[idf])
            wr = k.sb("wr", [128, KC, NEXP])
            k.dma("sp", wr[:, :, :], c["router_w"][:, :].rearrange("(j p) n -> p j n", p=128), wr, writes=[wr])
            brb = k.sb("brb", [128, NEXP])
            load_bc(k, "sp", brb, c["router_b"][0, :])
            hfT = k.sb("hfT", [128, KC, 128])
            tpf = RR([k.ps(f"tpf{i}", [128, 4, 128]) for i in range(1)])
            plg = k.ps("plg", [128, 512])
            lgs = RR([k.sb(f"lg{i}", [128, 64]) for i in range(2)])
        for t in range(NTILE):
            if t in (0, NCTX // 128):
                off = V_CTX if t == 0 else 0
                load_bc(k, "sp", gt1, vec[V_GT1 + off, :])
                load_bc(k, "sp", G2, vec[V_SC2 + off, :])
                load_bc(k, "sp", sh2, vec[V_SH2 + off, :])
                k.op("dve", lambda e: e.scalar_tensor_tensor(out=G2[:, :], in0=G2[:, :], scalar=1.0, in1=g2b[:, :],
                                                            op0=ALU.add, op1=ALU.mult), reads=[G2, g2b], writes=[G2])
            k.dma("sp", yt[:, :], c["Y"][t * 128:(t + 1) * 128, :], yt, writes=[yt])
            k.dma("pool", xt[:, :], c["x"][t * 128:(t + 1) * 128, :], xt, writes=[xt])
            for gi in range(4):
                k.op("act", lambda e, gi=gi, t=t: e.activation(out=tmp[:, 0:GW], in_=yt[:, gi * GW:(gi + 1) * GW], func=AF.Square,
                                                              accum_out=gs[:, 4 * t + gi:4 * t + gi + 1]),
                     reads=[yt, gs.r("z")], writes=[tmp, gs.r((t, gi))])
            rr = [gs.r((t, gi)) for gi in range(4)]
            rstd_from_ss(k, gs[:, 4 * t:4 * t + 4], gs[:, 4 * t:4 * t + 4], GW, rr, rr)
            for gi in range(4):
                k.op("dve", lambda e, gi=gi, t=t: e.scalar_tensor_tensor(
                    out=hb[:, gi * GW:(gi + 1) * GW], in0=yt[:, gi * GW:(gi + 1) * GW], scalar=gs[:, 4 * t + gi:4 * t + gi + 1],
                    in1=gbr[:, gi * GW:(gi + 1) * GW], op0=ALU.mult, op1=ALU.mult), reads=[yt, gs.r((t, gi)), gbr], writes=[hb])
            yT = yTs()
            for j0 in range(0, KC, 4):
                tp = tps()
                for jj in range(4):
                    j = j0 + jj
                    k.op("pe", lambda e, tp=tp, jj=jj, j=j: e.transpose(tp[:, jj, :], hb[:, j * 128:(j + 1) * 128], ident[:, :]),
                         reads=[hb, ident], writes=[tp])
                k.op(evac(), lambda e, tp=tp, j0=j0, yT=yT: _copy(e, yT[:, j0:j0 + 4, :], tp[:, 0:4, :]), reads=[tp], writes=[yT])
            for cb in range(4):
                pp = pps()
                mm_acc(k, pp[:, :], pp, [(yT[:, j, :], wo[:, j, cb * 512:(cb + 1) * 512]) for j in range(KC)], [yT, wo.r(cb)])
                k.op("dve", lambda e, pp=pp, cb=cb: e.tensor_tensor(tmc[:, :], pp[:, :], gt1[:, cb * 512:(cb + 1) * 512], ALU.mult),
                     reads=[pp, gt1], writes=[tmc])
                k.op("pool", lambda e, cb=cb: e.tensor_tensor(x1[:, cb * 512:(cb + 1) * 512], tmc[:, :], xt[:, cb * 512:(cb + 1) * 512], ALU.add),
                     reads=[tmc, xt], writes=[x1])
            k.dma("sp", c["X1"][t * 128:(t + 1) * 128, :], x1[:, :], x1, reads=[x1])
            h2 = h2s()
            norm_mod_T(k, x1, G2, sh2, ss2, t, tmp, hb, lambda j0, n, h2=h2: (h2[:, j0:j0 + n, :], [h2]), tps, ident, evac)
            k.dma("pool", c["h2T"][:, :, t * 128:(t + 1) * 128], h2[:, :, :], h2, reads=[h2])
            if moe:
                for j0 in range(0, KC, 4):
                    tp = tpf()
                    for jj in range(4):
                        j = j0 + jj
                        k.op("pe", lambda e, tp=tp, jj=jj, j=j: e.transpose(tp[:, jj, :], tmp[:, j * 128:(j + 1) * 128], idf[:, :]),
                             reads=[tmp, idf], writes=[tp])
                    k.op(evac(), lambda e, tp=tp, j0=j0: _copy(e, hfT[:, j0:j0 + 4, :], tp[:, 0:4, :]), reads=[tp], writes=[hfT])
                mm_acc(k, plg[:, 0:NEXP], plg, [(hfT[:, j, :], wr[:, j, :]) for j in range(KC)], [hfT, wr])
                router_tile(k, lgs(), plg, brb, c, t)


def stage_wcast(k, c):
    with k.stage():
        stg = RR([k.sb(f"cst{i}", [128, DFF]) for i in range(2)])
        wbf = RR([k.sb(f"cbf{i}", [128, DFF], BF16) for i in range(2)])
        cast = RR(["dve", "pool", "act"])
        dq = RR(["sp", "pool"])
        for nm, dst in (("w1", "wb1"), ("w3", "wb3")):
            for j in range(KC):
                st, wb = stg(), wbf()
                k.dma(dq(), st[:, :], c[nm][j * 128:(j + 1) * 128, :], st, writes=[st])
                k.op(cast(), lambda e, st=st, wb=wb: _copy(e, wb[:, :], st[:, :]), reads=[st], writes=[wb])
                k.dma(dq(), c[dst][:, :, j, :].rearrange("f p n -> p f n"), wb[:, :].rearrange("p (f n) -> p f n", n=256), wb, reads=[wb])
        for j in range(FC):
            jb, q = j // 4, j % 4
            st, wb = stg(), wbf()
            k.dma(dq(), st[:, 0:D], c["w2"][j * 128:(j + 1) * 128, :], st, writes=[st])
            k.op(cast(), lambda e, st=st, wb=wb: _copy(e, wb[:, 0:D], st[:, 0:D]), reads=[st], writes=[wb])
            k.dma(dq(), c["wb2"][:, jb, :, q, :].rearrange("c p n -> p c n"), wb[:, 0:D].rearrange("p (c n) -> p c n", n=512), wb, reads=[wb])


def stage_ffn(k, c, ntok, expert):
    c["wb1"] = k.dram("wb1", [FC // 2, 128, KC, 256], BF16)
    c["wb3"] = k.dram("wb3", [FC // 2, 128, KC, 256], BF16)
    c["wb2"] = k.dram("wb2", [4, FC // 4, 128, 4, 512], BF16)
    stage_wcast(k, c)
    with k.stage():
        G = 512
        hid = k.sb("hid", [128, FC, G], BF16)
        h2g = RR([k.sb(f"h2g{i}", [128, KC, G], BF16) for i in range(2)])
        w1s = RR([k.sb(f"w1b{i}", [128, KC, 256], BF16) for i in range(3)])
        w3s = RR([k.sb(f"w3b{i}", [128, KC, 256], BF16) for i in range(3)])
        w2s = RR([k.sb(f"w2b{i}", [128, 4, 512], BF16) for i in range(4)])
        sas = RR([k.sb(f"sa{i}", [128, G]) for i in range(2)])
        obs = RR([k.sb(f"ob{i}", [128, 512]) for i in range(3)])
        pas = RR([k.ps(f"pa{i}", [128, 512]) for i in range(2)])
        pbs = RR([k.ps(f"pb{i}", [128, 512]) for i in range(2)])
        pos = [k.ps(f"po{i}", [128, 512]) for i in range(4)]
        dq = RR(["sp", "pool"])
        ntile = ntok // 128
        if expert:
            gcol = k.sb("gcol", [128, ntile])
            k.dma("sp", gcol[:, :], c["gcol"][:, :], gcol, writes=[gcol])
        else:
            gt2 = [k.sb(f"gt2{i}", [128, D]) for i in range(2)]
            load_bc(k, "sp", gt2[0], c["vec"][V_GT2 + V_CTX, :])
            load_bc(k, "sp", gt2[1], c["vec"][V_GT2, :])
            x1s = RR([k.sb(f"x1c{i}", [128, 512]) for i in range(3)])
        for g0 in range(0, ntok, G):
            gsz = min(G, ntok - g0)
            hg = h2g()
            k.dma("sp", hg[:, :, 0:gsz], c["h2T"][:, :, g0:g0 + gsz], hg, writes=[hg])
            for fb in range(FC // 2):
                w1b, w3b = w1s(), w3s()
                k.dma("sp", w1b[:, :, :], c["wb1"][fb, :, :, :], w1b, writes=[w1b])
                k.dma("pool", w3b[:, :, :], c["wb3"][fb, :, :, :], w3b, writes=[w3b])
                for jj in range(2):
                    j = fb * 2 + jj
                    pa, pb = pas(), pbs()
                    mm_acc(k, pa[:, 0:gsz], pa, [(w1b[:, kc, jj * 128:(jj + 1) * 128], hg[:, kc, 0:gsz]) for kc in range(KC)], [w1b, hg])
                    mm_acc(k, pb[:, 0:gsz], pb, [(w3b[:, kc, jj * 128:(jj + 1) * 128], hg[:, kc, 0:gsz]) for kc in range(KC)], [w3b, hg])
                    sa = sas()
                    k.op("act", lambda e, sa=sa, pa=pa, gsz=gsz: e.activation(out=sa[:, 0:gsz], in_=pa[:, 0:gsz], func=AF.Silu), reads=[pa], writes=[sa])
                    k.op("dve", lambda e, sa=sa, pb=pb, j=j, gsz=gsz: e.tensor_tensor(hid[:, j, 0:gsz], sa[:, 0:gsz], pb[:, 0:gsz], ALU.mult),
                         reads=[sa, pb], writes=[hid.r(j)])
            nsub = gsz // 128
            for cb in range(4):
                for jb in range(FC // 4):
                    w2b = w2s()
                    k.dma(dq(), w2b[:, :, :], c["wb2"][cb, jb, :, :, :], w2b, writes=[w2b])
                    for sub in range(nsub):
                        for q in range(4):
                            j = jb * 4 + q
                            first = (jb == 0 and q == 0)
                            last = (jb == FC // 4 - 1 and q == 3)
                            k.op("pe", lambda e, sub=sub, j=j, q=q, w2b=w2b, first=first, last=last: e.matmul(
                                pos[sub][:, :], hid[:, j, sub * 128:(sub + 1) * 128], w2b[:, q, :], start=first, stop=last),
                                reads=[hid.r(j), w2b], writes=[pos[sub]], acc=not first)
                for sub in range(nsub):
                    tile = (g0 + sub * 128) // 128
                    r0 = g0 + sub * 128
                    ob = obs()
                    if expert:
                        k.op("act", lambda e, ob=ob, sub=sub, tile=tile: e.activation(out=ob[:, :], in_=pos[sub][:, :], func=AF.Copy,
                                                                                   scale=gcol[:, tile:tile + 1]), reads=[pos[sub], gcol], writes=[ob])
                    else:
                        gv = gt2[0] if tile < NCTX // 128 else gt2[1]
                        xc = x1s()
                        k.dma("sp", xc[:, :], c["X1"][r0:r0 + 128, cb * 512:(cb + 1) * 512], xc, writes=[xc])
                        k.op("dve", lambda e, ob=ob, sub=sub, gv=gv, cb=cb: e.tensor_tensor(ob[:, :], pos[sub][:, :], gv[:, cb * 512:(cb + 1) * 512], ALU.mult),
                             reads=[pos[sub], gv], writes=[ob])
                        k.op("pool", lambda e, ob=ob, xc=xc: e.tensor_tensor(ob[:, :], ob[:, :], xc[:, :], ALU.add), reads=[ob, xc], writes=[ob])
                    k.dma(dq(), c["out"][r0:r0 + 128, cb * 512:(cb + 1) * 512], ob[:, :], ob, reads=[ob])


B_INPUTS = [("x", [NTOK, D], F32), ("vec", [15, D], F32), ("w_in", [D, IN_W], F32), ("w_out", [D, D], F32),
            ("conv_pw", [GW, GW], F32), ("cpar", [128, 136], F32), ("pvec", [3, GW], F32), ("sgw", [4, 128, 128], F32),
            ("bsT", [128, 4], F32), ("gains", [4, 128], F32), ("sink", [1, 4], F32), ("rope", [TPC, 128], F32),
            ("KTs", [128, 2, NTOK], BF16), ("Vs", [NTOK, 2, 129], BF16), ("KTg", [128, 2, SEQ], BF16), ("Vg", [SEQ, 2, 129], BF16),
            ("uh", [2, HALO, GW], F32), ("masks", [4, 128, 128], BF16), ("ident", [128, 128], BF16), ("identf", [128, 128], F32)]


def build_B(moe, dbg=False):
    nc = new_nc()
    k = KB(nc)
    c = {}
    for name, shape, dt in B_INPUTS:
        c[name] = k.dram(name, shape, dt, kind="ExternalInput")
    kind_dbg = "ExternalOutput" if dbg else "Internal"
    c["P"] = k.dram("P", [NTOK, IN_W], F32, kind=kind_dbg)
    c["Y"] = k.dram("Y", [NTOK, D], F32, kind=kind_dbg)
    if moe:
        c["router_w"] = k.dram("router_w", [D, NEXP], F32, kind="ExternalInput")
        c["router_b"] = k.dram("router_b", [1, NEXP], F32, kind="ExternalInput")
        c["X1"] = k.dram("X1", [NTOK, D], F32, kind="ExternalOutput")
        c["h2T"] = k.dram("h2T", [128, KC, NTOK], BF16, kind="ExternalOutput")
        c["gates"] = k.dram("gates", [NTOK, NEXP], F32, kind="ExternalOutput")
    else:
        for nm, shp in (("w1", [D, DFF]), ("w3", [D, DFF]), ("w2", [DFF, D])):
            c[nm] = k.dram(nm, shp, F32, kind="ExternalInput")
        c["X1"] = k.dram("X1", [NTOK, D], F32, kind=kind_dbg)
        c["h2T"] = k.dram("h2T", [128, KC, NTOK], BF16, kind=kind_dbg)
        c["out"] = k.dram("out", [NTOK, D], F32, kind="ExternalOutput")
    stage_proj(k, c)
    stage_conv(k, c)
    stage_sgu(k, c)
    stage_attn(k, c)
    stage_out(k, c, moe)
    if not moe:
        stage_ffn(k, c, NTOK, expert=False)
    k.emit()
    return nc


NALL = NCTX + SEQ


def build_E(ntok=NALL):
    nc = new_nc()
    k = KB(nc)
    c = {"h2T": k.dram("h2T", [128, KC, ntok], BF16, kind="ExternalInput"),
         "gcol": k.dram("gcol", [128, ntok // 128], F32, kind="ExternalInput"),
         "w1": k.dram("w1", [D, DFF], F32, kind="ExternalInput"),
         "w3": k.dram("w3", [D, DFF], F32, kind="ExternalInput"),
         "w2": k.dram("w2", [DFF, D], F32, kind="ExternalInput"),
         "out": k.dram("out", [ntok, D], F32, kind="ExternalOutput")}
    stage_ffn(k, c, ntok, expert=True)
    k.emit()
    return nc


def build_C(nparts=NEXP):
    nc = new_nc()
    k = KB(nc)
    X1 = k.dram("X1", [NTOK, D], F32, kind="ExternalInput")
    part = k.dram("part", [nparts * NTOK, D], F32, kind="ExternalInput")
    vec2 = k.dram("vec2", [2, D], F32, kind="ExternalInput")
    out = k.dram("out", [NTOK, D], F32, kind="ExternalOutput")
    gt2 = [k.sb(f"gt2{i}", [128, D]) for i in range(2)]
    for i in range(2):
        load_bc(k, "sp", gt2[i], vec2[i, :])
    pts = RR([k.sb(f"pt{i}", [128, D]) for i in range(10)])
    xts = RR([k.sb(f"xt{i}", [128, D]) for i in range(2)])
    dq = RR(["sp", "pool"])
    eng = RR(["dve", "pool"])
    for t in range(NTILE):
        xt = xts()
        k.dma(dq(), xt[:, :], X1[t * 128:(t + 1) * 128, :], xt, writes=[xt])
        ps_ = []
        for e_ in range(nparts):
            p = pts()
            k.dma(dq(), p[:, :], part[e_ * NTOK + t * 128:e_ * NTOK + (t + 1) * 128, :], p, writes=[p])
            ps_.append(p)
        for a, b in (((0, 1), (2, 3), (4, 5), (6, 7), (0, 2), (4, 6), (0, 4)) if nparts == 8 else ((0, 1),)):
            k.op(eng(), lambda e, a=ps_[a], b=ps_[b]: e.tensor_tensor(a[:, :], a[:, :], b[:, :], ALU.add), reads=[ps_[a], ps_[b]], writes=[ps_[a]])
        gv = gt2[0] if t < NCTX // 128 else gt2[1]
        s0 = ps_[0]
        k.op("dve", lambda e, s0=s0, gv=gv: e.tensor_tensor(s0[:, :], s0[:, :], gv[:, :], ALU.mult), reads=[s0, gv], writes=[s0])
        k.op("pool", lambda e, s0=s0, xt=xt: e.tensor_tensor(s0[:, :], s0[:, :], xt[:, :], ALU.add), reads=[s0, xt], writes=[s0])
        k.dma(dq(), out[t * 128:(t + 1) * 128, :], s0[:, :], s0, reads=[s0])
    k.emit()
    return nc


_NC_CACHE = {}


def _get_nc(name, fn):
    if name not in _NC_CACHE:
        _NC_CACHE[name] = fn()
    return _NC_CACHE[name]


def _run(name, fn, maps):
    nc = _get_nc(name, fn)
    return run_bass_kernel_spmd(nc, maps, core_ids=list(range(NCORES))).results


def _c(a):
    return np.ascontiguousarray(a)


def kernel(**inp):
    f32 = np.float32
    inp = {k_: np.asarray(v) for k_, v in inp.items()}
    m = run_ada(inp)
    x_lat = _c(inp["x"][0]).astype(f32, copy=False)
    x_ctx = _c(inp["ctx"][0]).astype(f32, copy=False)
    rope = rope_table()
    ident = np.eye(128, dtype=f32).astype(NPBF)
    identf = np.eye(128, dtype=f32)
    kq = np.arange(128)
    tri_prev = (kq[:, None] >= kq[None, :]).astype(f32)
    tri_next = (kq[:, None] <= kq[None, :]).astype(f32)
    zero = np.zeros((128, 128), f32)
    for l in range(DEPTH):
        moe = (l % 2 == 1)
        sh1, sc1, gt1, sh2, sc2, gt2 = m[l, 0].reshape(6, D)
        csh1, csc1, cgt1, csh2, csc2, cgt2 = m[l, 1].reshape(6, D)
        vec = _c(np.stack([inp["g_norm1"][l], sc1, sh1, gt1, sc2, sh2, gt2, csc1, csh1, cgt1, csc2, csh2, cgt2,
                           inp["g_branch"][l], inp["g_norm2"][l]]).astype(f32))
        w_in = inp["w_in"][l]
        wkv = _c(np.concatenate([w_in[:, 2560:3072], w_in[:, 3584:4096]], axis=1))
        wab = _c(w_in[:, 0:1024])
        kg = _c(np.stack([inp["swa_k_g"][l], inp["glb_k_g"][l]]))
        maps = [{"x": x_lat[c * TPC:(c + 1) * TPC], "vec": _c(vec[0:3]), "wkv": wkv, "wab": wab, "kg": kg,
                 "rope": rope[c * TPC:(c + 1) * TPC], "ident": ident} for c in range(NCORES)]
        ra = _run("A", build_A, maps)
        KTg = _c(np.concatenate([ra[c]["kT"][:, 1] for c in range(NCORES)], axis=2))
        Vg = np.ones((SEQ, 2, 129), NPBF)
        Vg[:, :, :128] = np.concatenate([ra[c]["v"][:, 1].reshape(TPC, 2, 128) for c in range(NCORES)], axis=0)
        maps = []
        for c in range(NCORES):
            KTs = np.zeros((128, 2, NTOK), NPBF)
            Vs = np.ones((NTOK, 2, 129), NPBF)
            Vs[:, :, :128] = 0
            uh = np.zeros((2, HALO, GW), f32)
            KTs[:, :, 128:128 + TPC] = ra[c]["kT"][:, 0]
            Vs[128:128 + TPC, :, :128] = ra[c]["v"][:, 0].reshape(TPC, 2, 128)
            if c > 0:
                KTs[:, :, 0:128] = ra[c - 1]["kT"][:, 0][:, :, TPC - 128:]
                Vs[0:128, :, :128] = ra[c - 1]["v"][TPC - 128:, 0].reshape(128, 2, 128)
                uh[0] = ra[c - 1]["u"][1][128 - HALO:]
            if c < NCORES - 1:
                KTs[:, :, 128 + TPC:] = ra[c + 1]["kT"][:, 0][:, :, 0:128]
                Vs[128 + TPC:, :, :128] = ra[c + 1]["v"][0:128, 0].reshape(128, 2, 128)
                uh[1] = ra[c + 1]["u"][0][0:HALO]
            masks = np.stack([tri_prev, tri_next, zero if c == 0 else tri_prev, zero if c == NCORES - 1 else tri_next]).astype(NPBF)
            cw = inp["conv_w"][l]
            cpar = np.concatenate([cw.T.reshape(4, 128, CONV_W).transpose(1, 0, 2).reshape(128, 4 * CONV_W),
                                   inp["conv_b"][l].reshape(4, 128).T, inp["conv_ln_g"][l].reshape(4, 128).T,
                                   inp["conv_ln_b"][l].reshape(4, 128).T], axis=1)
            d = {"x": _c(np.concatenate([x_ctx, x_lat[c * TPC:(c + 1) * TPC]], axis=0)), "vec": vec, "w_in": w_in,
                 "w_out": inp["w_out"][l], "conv_pw": inp["conv_pw"][l], "cpar": _c(cpar.astype(f32)),
                 "pvec": _c(np.stack([inp["conv_pw_b"][l], inp["sgu_ln_g"][l], inp["sgu_ln_b"][l]])),
                 "sgw": _c(inp["sgu_w"][l].transpose(0, 2, 1)), "bsT": _c(inp["sgu_b"][l].T),
                 "gains": _c(np.stack([inp["swa_q_g"][l], inp["swa_k_g"][l], inp["glb_q_g"][l], inp["glb_k_g"][l]])),
                 "sink": _c(inp["swa_sink"][l][None, :]), "rope": rope[c * TPC:(c + 1) * TPC],
                 "KTs": KTs, "Vs": Vs, "KTg": KTg, "Vg": Vg, "uh": uh, "masks": masks, "ident": ident, "identf": identf}
            if moe:
                d["router_w"] = inp["router_w"][l // 2]
                d["router_b"] = _c(inp["router_b"][l // 2][None, :])
            else:
                d["w1"], d["w3"], d["w2"] = inp["ffn_w1"][l // 2], inp["ffn_w3"][l // 2], inp["ffn_w2"][l // 2]
            maps.append(d)
        del ra
        if not moe:
            rb = _run("Bd", lambda: build_B(False), maps)
            del maps
            x_ctx = _c(rb[0]["out"][:NCTX])
            x_lat = _c(np.concatenate([rb[c]["out"][NCTX:] for c in range(NCORES)], axis=0))
            del rb
            continue
        rb = _run("Bm", lambda: build_B(True), maps)
        del maps
        h2T = _c(np.concatenate([rb[0]["h2T"][:, :, :NCTX]] + [rb[c]["h2T"][:, :, NCTX:] for c in range(NCORES)], axis=2))
        gates = np.concatenate([rb[0]["gates"][:NCTX]] + [rb[c]["gates"][NCTX:] for c in range(NCORES)], axis=0)
        X1 = [rb[c]["X1"] for c in range(NCORES)]
        del rb
        sel = gates > 0
        idxs = [np.nonzero(sel[:, e])[0] for e in range(NEXP)]
        cap = max(128, -(-max(len(ix) for ix in idxs) // 128) * 128)
        maps = []
        for e in range(NEXP):
            n_e = len(idxs[e])
            h_e = np.zeros((128, KC, cap), NPBF)
            h_e[:, :, :n_e] = h2T[:, :, idxs[e]]
            g_e = np.zeros((cap,), f32)
            g_e[:n_e] = gates[idxs[e], e]
            maps.append({"h2T": h_e, "gcol": _c(g_e.reshape(cap // 128, 128).T),
                         "w1": inp["exp_w1"][l // 2][e], "w3": inp["exp_w3"][l // 2][e], "w2": inp["exp_w2"][l // 2][e]})
        re_ = _run(("E", cap), lambda: build_E(cap), maps)
        del maps, h2T
        outs = [re_[e]["out"] for e in range(NEXP)]
        del re_
        order = np.argsort(~sel, axis=1, kind="stable")[:, :2]
        parts_all = np.zeros((2, NALL, D), f32)
        for e in range(NEXP):
            inv = np.full((NALL,), -1, np.int64)
            inv[idxs[e]] = np.arange(len(idxs[e]))
            for s_ in range(2):
                tok = np.nonzero((order[:, s_] == e) & sel[:, e])[0]
                parts_all[s_, tok] = outs[e][inv[tok]]
        vec2 = _c(np.stack([cgt2, gt2]).astype(f32))
        maps = []
        for c in range(NCORES):
            part = np.concatenate([np.concatenate([parts_all[s_, :NCTX], parts_all[s_, NCTX + c * TPC:NCTX + (c + 1) * TPC]], axis=0)
                                   for s_ in range(2)], axis=0)
            maps.append({"X1": X1[c], "part": part, "vec2": vec2})
        del parts_all
        del outs
        rc = _run("C2", lambda: build_C(2), maps)
        del maps
        x_ctx = _c(rc[0]["out"][:NCTX])
        x_lat = _c(np.concatenate([rc[c]["out"][NCTX:] for c in range(NCORES)], axis=0))
        del rc
    return x_lat[None].astype(f32)
```
